# Optimizing a Trainium2 kernel written in Bass

```python
import jax, jax.numpy as jnp
from jax import lax
import numpy as np

D_MODEL = 2048
BATCH = 1
SEQ = 8192
DEPTH = 2

HEAD_DIM = 128
N_HEADS_A = 6
N_HEADS_B = 6
N_HEADS_C = 4
W_A = N_HEADS_A * HEAD_DIM
W_B = N_HEADS_B * HEAD_DIM
W_C = N_HEADS_C * HEAD_DIM
Q_RANK_A = 512
KV_RANK_A = 256
N_IDX_HEADS = 16
IDX_DIM = 64
TOPK_A_MAX = 256
MOBA_BLOCK = 256
MOBA_TOPK = 3
N_MEM = 256
N_ALIBI = N_HEADS_A + N_HEADS_B
N_GROUPS = 4
EXPERTS_PER_GROUP = 8
N_EXPERTS = N_GROUPS * EXPERTS_PER_GROUP
TOPK_IN_GROUP = 2
D_EXPERT = 512
Q_BLOCK = 128
ALPHA = (2 * DEPTH) ** 0.25
BETA = (8 * DEPTH) ** -0.25
NORM_EPS = 1e-5
NEG = -1e30

kernel_name = 'hybrid_dsa_moba_mem_hmoe_deepnorm'


def layer_norm(x, g, b):
    xf = x.astype(jnp.float32)
    mu = jnp.mean(xf, -1, keepdims=True)
    var = jnp.mean(jnp.square(xf - mu), -1, keepdims=True)
    return ((xf - mu) * lax.rsqrt(var + NORM_EPS) * g.astype(jnp.float32) + b.astype(jnp.float32)).astype(x.dtype)


def rms_norm(x, g):
    xf = x.astype(jnp.float32)
    return (xf * lax.rsqrt(jnp.mean(jnp.square(xf), -1, keepdims=True) + NORM_EPS) * g.astype(jnp.float32)).astype(x.dtype)


def split_cols(h):
    sizes = [Q_RANK_A, KV_RANK_A, IDX_DIM, N_IDX_HEADS, W_B, W_B, W_B, W_C]
    return jnp.split(h, np.cumsum(sizes)[:-1].tolist(), axis=-1)


def dsa_attention(q, k, v, q_idx, k_idx, w_idx, slopes):
    B, T = q.shape[:2]
    top_k = min(TOPK_A_MAX, T // 4)
    s_pos = jnp.arange(T)

    def one_block(i):
        t0 = i * Q_BLOCK
        t_pos = t0 + jnp.arange(Q_BLOCK)
        qi = lax.dynamic_slice_in_dim(q_idx, t0, Q_BLOCK, axis=1)
        wi = lax.dynamic_slice_in_dim(w_idx, t0, Q_BLOCK, axis=1)
        rel = jax.nn.relu(jnp.einsum('bqhd,bsd->bqhs', qi, k_idx))
        score = jnp.einsum('bqh,bqhs->bqs', wi, rel).astype(jnp.float32)
        score = jnp.where((s_pos[None, :] <= t_pos[:, None])[None], score, NEG)
        _, sel = lax.top_k(score, top_k)
        ks = jax.vmap(lambda kk, ii: kk[ii])(k, sel)
        vs = jax.vmap(lambda vv, ii: vv[ii])(v, sel)
        qb = lax.dynamic_slice_in_dim(q, t0, Q_BLOCK, axis=1)
        s = jnp.einsum('bqhd,bqkhd->bhqk', qb, ks).astype(jnp.float32) * HEAD_DIM ** -0.5
        dist = (t_pos[None, :, None] - sel).astype(jnp.float32)
        s = s - slopes[None, :, None, None] * dist[:, None]
        s = jnp.where((sel <= t_pos[None, :, None])[:, None], s, NEG)
        p = jax.nn.softmax(s, axis=-1).astype(v.dtype)
        return jnp.einsum('bhqk,bqkhd->bqhd', p, vs)

    o = lax.map(one_block, jnp.arange(T // Q_BLOCK))
    return jnp.moveaxis(o, 0, 1).reshape(B, T, -1)


def moba_attention(q, k, v, slopes):
    B, T, H, Dh = q.shape
    n_kb = -(-T // MOBA_BLOCK)
    t_pad = n_kb * MOBA_BLOCK
    top_b = min(MOBA_TOPK, n_kb)
    pad = ((0, 0), (0, t_pad - T), (0, 0), (0, 0))
    k_p, v_p = jnp.pad(k, pad), jnp.pad(v, pad)
    k_blk = k_p.reshape(B, n_kb, MOBA_BLOCK, H, Dh)
    k_mean = jnp.mean(k_blk, axis=2)
    k_bh = k_blk.transpose(0, 3, 1, 2, 4)
    v_bh = v_p.reshape(B, n_kb, MOBA_BLOCK, H, Dh).transpose(0, 3, 1, 2, 4)
    h_ix = jnp.arange(H)[None, :, None]
    off = jnp.arange(MOBA_BLOCK)
    scale = Dh ** -0.5

    def one_block(i):
        t0 = i * Q_BLOCK
        t_pos = t0 + jnp.arange(Q_BLOCK)
        c = t0 // MOBA_BLOCK
        qb = lax.dynamic_slice_in_dim(q, t0, Q_BLOCK, axis=1)
        gate = jnp.einsum('bqhd,bnhd->bqhn', qb, k_mean).astype(jnp.float32)
        gate = jnp.where(jnp.arange(n_kb) < c, gate, NEG)
        _, blk = lax.top_k(gate, top_b)
        ks = jax.vmap(lambda kk, ii: kk[h_ix, ii])(k_bh, blk)
        vs = jax.vmap(lambda vv, ii: vv[h_ix, ii])(v_bh, blk)
        s_sel = jnp.einsum('bqhd,bqhjkd->bqhjk', qb, ks).astype(jnp.float32) * scale
        pos_sel = blk[..., None] * MOBA_BLOCK + off
        dist_sel = (t_pos[None, :, None, None, None] - pos_sel).astype(jnp.float32)
        s_sel = s_sel - slopes[None, None, :, None, None] * dist_sel
        s_sel = jnp.where((blk < c)[..., None], s_sel, NEG)
        k_own = lax.dynamic_slice_in_dim(k_p, c * MOBA_BLOCK, MOBA_BLOCK, axis=1)
        v_own = lax.dynamic_slice_in_dim(v_p, c * MOBA_BLOCK, MOBA_BLOCK, axis=1)
        s_own = jnp.einsum('bqhd,bkhd->bqhk', qb, k_own).astype(jnp.float32) * scale
        dist_own = t_pos[:, None] - (c * MOBA_BLOCK + off)[None, :]
        s_own = s_own - slopes[None, None, :, None] * dist_own.astype(jnp.float32)[None, :, None, :]
        s_own = jnp.where((dist_own >= 0)[None, :, None, :], s_own, NEG)
        s = jnp.concatenate([s_sel.reshape(B, Q_BLOCK, H, top_b * MOBA_BLOCK), s_own], axis=-1)
        p = jax.nn.softmax(s, axis=-1).astype(v.dtype)
        p_sel = p[..., :top_b * MOBA_BLOCK].reshape(B, Q_BLOCK, H, top_b, MOBA_BLOCK)
        p_own = p[..., top_b * MOBA_BLOCK:]
        return (jnp.einsum('bqhjk,bqhjkd->bqhd', p_sel, vs)
                + jnp.einsum('bqhk,bkhd->bqhd', p_own, v_own))

    o = lax.map(one_block, jnp.arange(T // Q_BLOCK))
    return jnp.moveaxis(o, 0, 1).reshape(B, T, -1)


def memory_attention(q, mk, mv):
    B, T = q.shape[:2]
    s = jnp.einsum('bthd,bmhd->bhtm', q, mk).astype(jnp.float32) * HEAD_DIM ** -0.5
    p = jax.nn.softmax(s, axis=-1).astype(mv.dtype)
    return jnp.einsum('bhtm,bmhd->bthd', p, mv).reshape(B, T, -1)


def hier_moe(x, w_grp, b_grp, w_rt, b_rt, w1, w3, w2):
    B, T, D = x.shape
    xt = x.reshape(B * T, D)
    n = xt.shape[0]
    g_logit = (xt @ w_grp + b_grp).astype(jnp.float32)
    g_sel = jnp.argmax(g_logit, axis=-1)
    p_g = jnp.take_along_axis(jax.nn.softmax(g_logit, -1), g_sel[:, None], -1)
    e_logit = (xt @ w_rt + b_rt).astype(jnp.float32).reshape(n, N_GROUPS, EXPERTS_PER_GROUP)
    e_logit = jnp.take_along_axis(e_logit, g_sel[:, None, None], axis=1)[:, 0]
    p_e, e_sel = lax.top_k(jax.nn.softmax(e_logit, -1), TOPK_IN_GROUP)
    p_e = p_e / jnp.sum(p_e, -1, keepdims=True)
    wts = p_g * p_e
    gidx = g_sel[:, None] * EXPERTS_PER_GROUP + e_sel
    comb = jnp.einsum('nk,nke->ne', wts, jax.nn.one_hot(gidx, N_EXPERTS, dtype=jnp.float32)).astype(x.dtype)
    out = jnp.zeros_like(xt)
    for e in range(N_EXPERTS):
        hid = jax.nn.silu(xt @ w1[e]) * (xt @ w3[e])
        out = out + comb[:, e:e + 1] * (hid @ w2[e])
    return out.reshape(B, T, D)


def setup_inputs(seed: int = 0) -> dict:
    key = jax.random.key(seed)
    keys = iter(jax.random.split(key, 64))

    def nrm(shape, fan_in, scale=1.0):
        return jax.random.normal(next(keys), shape, jnp.float32) * (scale * fan_in ** -0.5)

    def gain(shape):
        return 1.0 + 0.05 * jax.random.normal(next(keys), shape, jnp.float32)

    def bias(shape):
        return 0.01 * jax.random.normal(next(keys), shape, jnp.float32)

    L, D = DEPTH, D_MODEL
    x = jax.random.normal(next(keys), (BATCH, SEQ, D), jnp.float32)
    mem = jax.random.normal(next(keys), (BATCH, N_MEM, D), jnp.float32)
    w_mem_kv = jnp.concatenate([nrm((D, W_C), D), nrm((D, W_C), D, BETA)], -1)
    w_in = jnp.concatenate([
        nrm((L, D, Q_RANK_A), D), nrm((L, D, KV_RANK_A), D), nrm((L, D, IDX_DIM), D),
        nrm((L, D, N_IDX_HEADS), D), nrm((L, D, W_B), D), nrm((L, D, W_B), D),
        nrm((L, D, W_B), D, BETA), nrm((L, D, W_C), D)], -1)
    return {
        'x': x,
        'mem': mem,
        'w_mem_kv': w_mem_kv,
        'w_in': w_in,
        'g_cq': gain((L, Q_RANK_A)),
        'g_ckv': gain((L, KV_RANK_A)),
        'g_kidx': gain((L, IDX_DIM)),
        'b_kidx': bias((L, IDX_DIM)),
        'w_uq': nrm((L, Q_RANK_A, W_A), Q_RANK_A),
        'w_uqi': nrm((L, Q_RANK_A, N_IDX_HEADS * IDX_DIM), Q_RANK_A),
        'w_ukv': jnp.concatenate([nrm((L, KV_RANK_A, W_A), KV_RANK_A), nrm((L, KV_RANK_A, W_A), KV_RANK_A, BETA)], -1),
        'w_up_a': nrm((L, W_A, D), W_A, BETA),
        'w_up_b': nrm((L, W_B, D), W_B, BETA),
        'w_up_c': nrm((L, W_C, D), W_C, BETA),
        'w_gate': nrm((L, D, 3 * D), D),
        'b_gate': bias((L, 3 * D)),
        'w_o': nrm((L, D, D), D, BETA),
        'ln1_g': gain((L, D)),
        'ln1_b': bias((L, D)),
        'w_grp': nrm((L, D, N_GROUPS), D),
        'b_grp': bias((L, N_GROUPS)),
        'w_rt': nrm((L, D, N_EXPERTS), D),
        'b_rt': bias((L, N_EXPERTS)),
        'w1': nrm((L, N_EXPERTS, D, D_EXPERT), D),
        'w3': nrm((L, N_EXPERTS, D, D_EXPERT), D),
        'w2': nrm((L, N_EXPERTS, D_EXPERT, D), D_EXPERT, BETA),
        'ln2_g': gain((L, D)),
        'ln2_b': bias((L, D)),
    }


def reference(x, mem, w_mem_kv, w_in, g_cq, g_ckv, g_kidx, b_kidx, w_uq, w_uqi, w_ukv,
              w_up_a, w_up_b, w_up_c, w_gate, b_gate, w_o, ln1_g, ln1_b,
              w_grp, b_grp, w_rt, b_rt, w1, w3, w2, ln2_g, ln2_b):
    B, T, _ = x.shape
    slopes = 2.0 ** (-8.0 * jnp.arange(1, N_ALIBI + 1, dtype=jnp.float32) / N_ALIBI)
    slopes_a, slopes_b = slopes[0::2], slopes[1::2]
    mk, mv = jnp.split(mem @ w_mem_kv, 2, axis=-1)
    mk = mk.reshape(B, N_MEM, N_HEADS_C, HEAD_DIM)
    mv = mv.reshape(B, N_MEM, N_HEADS_C, HEAD_DIM)
    for l in range(DEPTH):
        h = x
        c_q, c_kv, k_i, w_i, q_b, k_b, v_b, q_c = split_cols(h @ w_in[l])
        c_q = rms_norm(c_q, g_cq[l])
        c_kv = rms_norm(c_kv, g_ckv[l])
        q_a = (c_q @ w_uq[l]).reshape(B, T, N_HEADS_A, HEAD_DIM)
        q_i = (c_q @ w_uqi[l]).reshape(B, T, N_IDX_HEADS, IDX_DIM) * IDX_DIM ** -0.5
        k_a, v_a = jnp.split(c_kv @ w_ukv[l], 2, axis=-1)
        k_a = k_a.reshape(B, T, N_HEADS_A, HEAD_DIM)
        v_a = v_a.reshape(B, T, N_HEADS_A, HEAD_DIM)
        k_i = layer_norm(k_i, g_kidx[l], b_kidx[l])
        w_i = w_i * N_IDX_HEADS ** -0.5
        o_a = dsa_attention(q_a, k_a, v_a, q_i, k_i, w_i, slopes_a)
        o_b = moba_attention(q_b.reshape(B, T, N_HEADS_B, HEAD_DIM), k_b.reshape(B, T, N_HEADS_B, HEAD_DIM),
                             v_b.reshape(B, T, N_HEADS_B, HEAD_DIM), slopes_b)
        o_c = memory_attention(q_c.reshape(B, T, N_HEADS_C, HEAD_DIM), mk, mv)
        g_a, g_b, g_c = jnp.split(jax.nn.sigmoid(h @ w_gate[l] + b_gate[l]), 3, axis=-1)
        merged = g_a * (o_a @ w_up_a[l]) + g_b * (o_b @ w_up_b[l]) + g_c * (o_c @ w_up_c[l])
        x = layer_norm(ALPHA * x + merged @ w_o[l], ln1_g[l], ln1_b[l])
        y = hier_moe(x, w_grp[l], b_grp[l], w_rt[l], b_rt[l], w1[l], w3[l], w2[l])
        x = layer_norm(ALPHA * x + y, ln2_g[l], ln2_b[l])
    return x
```

```python
import numpy as np
import ml_dtypes
from contextlib import ExitStack
import concourse.bass as bass
import concourse.mybir as mybir
from concourse.bass_utils import run_bass_kernel_spmd

F32 = mybir.dt.float32
BF16 = mybir.dt.bfloat16
AF = mybir.ActivationFunctionType
ALU = mybir.AluOpType
AX = mybir.AxisListType

NCORE = 8
D = 2048
T = 8192
TL = 1024
NT = 8
DEPTH = 2
NEG = -1.0e30
ALPHA = (2 * DEPTH) ** 0.25
EPS = 1e-5
N_BISECT = 27
SCALE = 128 ** -0.5


def unit_of(r, j):
    return [r, 15 - r, 16 + r, 31 - r][j]


class Sem:
    def __init__(self, h):
        self.h = h
        self.n = 0


class Res:
    __slots__ = ("w", "r")

    def __init__(self):
        self.w = None
        self.r = {}


class Tile:
    def __init__(self, t):
        self.t = t
        self.res = Res()
        self.sub = {}

    def __getitem__(self, k):
        return self.t[k]

    def R(self, key=None):
        if key is None:
            return self.res
        if key not in self.sub:
            self.sub[key] = Res()
        return self.sub[key]


class Eng:
    def __init__(self, kb, name, h):
        self.kb = kb
        self.name = name
        self.h = h
        self.c = None
        self.d = None
        self.seen = {}

    def csem(self):
        if self.c is None or self.c.n > 30000:
            self.c = Sem(self.kb.new_sem())
            self.kb.allsems.append(self.c)
        return self.c

    def dsem(self):
        if self.d is None or self.d.n > 30000:
            self.d = Sem(self.kb.new_sem())
            self.kb.allsems.append(self.d)
        return self.d


class KB:
    def __init__(self):
        self.nc = bass.Bass("TRN2", target_bir_lowering=False)
        self.es = ExitStack()
        self.allsems = []
        nc = self.nc
        self.E = {
            "pe": Eng(self, "pe", nc.tensor),
            "act": Eng(self, "act", nc.scalar),
            "dve": Eng(self, "dve", nc.vector),
            "pool": Eng(self, "pool", nc.gpsimd),
            "sp": Eng(self, "sp", nc.sync),
        }
        self.nid = 0
        self.ninst = 0

    def new_sem(self):
        self.nid += 1
        return self.es.enter_context(self.nc.semaphore("s%d" % self.nid))

    def dram(self, name, shape, dt, kind):
        t = self.nc.dram_tensor(name, list(shape), dt, kind=kind)
        return Tile(t)

    def sb(self, stack, name, shape, dt):
        self.nid += 1
        t = stack.enter_context(self.nc.sbuf_tensor("%s_%d" % (name, self.nid), list(shape), dt))
        return Tile(t)

    def ps(self, stack, name, shape, dt):
        self.nid += 1
        t = stack.enter_context(self.nc.psum_tensor("%s_%d" % (name, self.nid), list(shape), dt))
        return Tile(t)

    def op(self, e, fn, rd=(), wr=(), dma=False):
        E = self.E[e]
        deps = {}

        def add(tok):
            s, v = tok
            if deps.get(s, 0) < v:
                deps[s] = v

        for r in rd:
            if r.w is not None:
                add(r.w)
        for r in wr:
            if r.w is not None:
                add(r.w)
            for s, v in r.r.items():
                add((s, v))
        for s, v in deps.items():
            if e == "pe" and (not dma) and s is E.c:
                continue
            if E.seen.get(s, 0) >= v:
                continue
            E.h.wait_ge(s.h, v)
            E.seen[s] = v
        ins = fn(E.h)
        self.ninst += 1
        if dma:
            S = E.dsem()
            S.n += 16
            ins.then_inc(S.h, 16)
        else:
            S = E.csem()
            S.n += 1
            ins.then_inc(S.h, 1)
        tok = (S, S.n)
        for r in rd:
            if r.r.get(S, 0) < S.n:
                r.r[S] = S.n
        for r in wr:
            r.w = tok
            r.r = {}
        return ins

    def barrier(self):
        for E in self.E.values():
            for S in self.allsems:
                if S.n > 0 and E.seen.get(S, 0) < S.n:
                    E.h.wait_ge(S.h, S.n)
                    E.seen[S] = S.n

    def final_wait(self):
        E = self.E["sp"]
        for S in self.allsems:
            if S.n > 0 and E.seen.get(S, 0) < S.n:
                E.h.wait_ge(S.h, S.n)
                E.seen[S] = S.n


class Ring:
    def __init__(self, tiles):
        self.tiles = tiles
        self.i = 0

    def next(self):
        t = self.tiles[self.i % len(self.tiles)]
        self.i += 1
        return t


class Ctx:
    def __init__(self, kb, stack, consts):
        self.kb = kb
        nc = kb.nc
        self.banks = [kb.ps(stack, "bank", [128, 512], F32) for _ in range(8)]
        self.bank_i = 0
        self.slabs = Ring([kb.sb(stack, "slab", [128, 4096], BF16) for _ in range(4)])
        self.stage = Ring([kb.sb(stack, "stage", [128, 4096], F32) for _ in range(2)])
        self.cst = {}
        for name, (shape, dt) in consts.items():
            d = kb.dram(name, shape, dt, "ExternalInput")
            t = kb.sb(stack, name, shape, dt)
            kb.op("sp", lambda h, t=t, d=d: h.dma_start(out=t[:], in_=d[:]), wr=[t.R()], dma=True)
            self.cst[name] = t
        self.ident_bf = kb.sb(stack, "identbf", [128, 128], BF16)
        kb.op("dve", lambda h: h.tensor_copy(out=self.ident_bf[:], in_=self.cst["ident"][:]),
              rd=[self.cst["ident"].R()], wr=[self.ident_bf.R()])
        self.evac_i = 0

    def bank(self):
        b = self.banks[self.bank_i % 8]
        self.bank_i += 1
        return b

    def evac_eng(self):
        self.evac_i += 1
        return "act" if self.evac_i % 2 else "dve"

    def copy(self, eng, out_ap, in_ap, rd, wr):
        kb = self.kb
        if eng == "act":
            kb.op("act", lambda h: h.activation(out=out_ap, in_=in_ap, func=AF.Copy), rd=rd, wr=wr)
        else:
            kb.op(eng, lambda h: h.tensor_copy(out=out_ap, in_=in_ap), rd=rd, wr=wr)

    def load_slab(self, src_ap, nk, C):
        kb = self.kb
        assert nk * C <= 4096
        st = self.stage.next()
        sl = self.slabs.next()
        sv = st.t[:, 0:nk * C].rearrange("p (k c) -> p k c", c=C)
        kb.op("sp", lambda h: h.dma_start(out=sv, in_=src_ap.rearrange("(k p) c -> p k c", p=128)),
              wr=[st.R()], dma=True)
        self.cast_i = getattr(self, "cast_i", 0) + 1
        if self.cast_i % 2:
            kb.op("pool", lambda h: h.tensor_copy(out=sl.t[:, 0:nk * C], in_=st.t[:, 0:nk * C]),
                  rd=[st.R()], wr=[sl.R()])
        else:
            kb.op("act", lambda h: h.activation(out=sl.t[:, 0:nk * C], in_=st.t[:, 0:nk * C], func=AF.Copy),
                  rd=[st.R()], wr=[sl.R()])
        return sl, sl.t[:, 0:nk * C].rearrange("p (k c) -> p k c", c=C)

    def transpose_to(self, src_tile, src_aps, dst_fn, dst_res, dt, rows=128):
        kb = self.kb
        per = 4 if dt == F32 else 8
        ident = self.cst["ident"] if dt == F32 else self.ident_bf
        for g0 in range(0, len(src_aps), per):
            n = min(per, len(src_aps) - g0)
            b = self.bank()
            bv = b.t[:] if dt == F32 else b.t[:].bitcast(BF16)
            for q in range(n):
                kb.op("pe", lambda h, q=q: h.transpose(out=bv[0:rows, q * 128:(q + 1) * 128],
                                                        in_=src_aps[g0 + q], identity=ident[:]),
                      rd=[src_tile.R(), ident.R()], wr=[b.R()])
            self.copy(self.evac_eng(), dst_fn(g0, n),
                      bv[0:rows, 0:n * 128].rearrange("p (n t) -> p n t", t=128), [b.R()], [dst_res])


def W2(t, l):
    return t.t[l]


def phase_M(cx, stack, din):
    kb = cx.kb
    mkT = kb.sb(stack, "mkT", [128, 4, 256], BF16)
    mv = kb.sb(stack, "mv", [128, 2, 4, 129], BF16)
    with ExitStack() as st:
        memT = kb.sb(st, "memT", [128, 16, 256], BF16)
        mem_in = kb.sb(st, "memin", [128, 2048], F32)
        for mt in range(2):
            kb.op("sp", lambda h: h.dma_start(out=mem_in[:], in_=din["mem"].t[mt * 128:(mt + 1) * 128, :]),
                  wr=[mem_in.R()], dma=True)
            cx.transpose_to(mem_in, [mem_in[:, c * 128:(c + 1) * 128] for c in range(16)],
                            lambda g0, n: memT[:, g0:g0 + n, mt * 128:(mt + 1) * 128], memT.R(), F32)
        kb.op("pool", lambda h: h.memset(mv[:], 1.0), wr=[mv.R()])
        for cs in range(4):
            sl, sv = cx.load_slab(din["w_mem_kv"].t[:, cs * 256:(cs + 1) * 256], 16, 256)
            if cs < 2:
                for hh in range(2):
                    b = cx.bank()
                    for k in range(16):
                        kb.op("pe", lambda h: h.matmul(b.t[:, 0:256], lhsT=sv[:, k, hh * 128:(hh + 1) * 128],
                                                       rhs=memT[:, k, :], start=(k == 0), stop=(k == 15)),
                              rd=[sl.R(), memT.R()], wr=[b.R()])
                    cx.copy(cx.evac_eng(), mkT[:, cs * 2 + hh, :], b.t[:, 0:256], [b.R()], [mkT.R()])
            else:
                for mt in range(2):
                    b = cx.bank()
                    for k in range(16):
                        kb.op("pe", lambda h: h.matmul(b.t[:, 0:256], lhsT=memT[:, k, mt * 128:(mt + 1) * 128],
                                                       rhs=sv[:, k, :], start=(k == 0), stop=(k == 15)),
                              rd=[sl.R(), memT.R()], wr=[b.R()])
                    h0 = (cs - 2) * 2
                    cx.copy(cx.evac_eng(), mv[:, mt, h0:h0 + 2, 0:128],
                            b.t[:, 0:256].rearrange("p (a d) -> p a d", d=128), [b.R()], [mv.R()])
    return mkT, mv


W_IN_SLABS = [(0, 256), (256, 256), (512, 256), (768, 80)] + \
             [(848 + 256 * i, 256) for i in range(3)] + [(1616 + 256 * i, 256) for i in range(3)] + \
             [(2384 + 256 * i, 256) for i in range(3)] + [(3152 + 256 * i, 256) for i in range(2)]


def phase_A(cx, l, xs_d, din, dq, dk):
    kb = cx.kb
    with ExitStack() as st:
        Hs = kb.sb(st, "Hs", [128, 8, 848], F32)
        fm = Ring([kb.sb(st, "fm", [128, 6, 1024], BF16) for _ in range(2)])
        vv = kb.sb(st, "vv", [128, 8, 6, 129], BF16)
        kb.op("pool", lambda h: h.memset(vv[:], 1.0), wr=[vv.R()])
        kms = kb.sb(st, "kms", [128, 4, 6], F32)
        kmb = kb.sb(st, "kmb", [128, 4, 6], BF16)
        st1 = ExitStack()
        xT = kb.sb(st1, "xT", [128, 16, 1024], BF16)
        qbT = fm.next(); kbT = fm.next(); qcT = None
        vb = vv; va = vv
        for i in range(NT):
            xi = cx.stage.next()
            kb.op("sp", lambda h: h.dma_start(out=xi[:, 0:2048], in_=xs_d.t[i * 128:(i + 1) * 128, :]),
                  rd=[xs_d.R()], wr=[xi.R()], dma=True)
            cx.transpose_to(xi, [xi[:, c * 128:(c + 1) * 128] for c in range(16)],
                            lambda g0, n: xT[:, g0:g0 + n, i * 128:(i + 1) * 128], xT.R(), F32)
        w_in = din["w_in"].t[0]
        for si, (c0, C) in enumerate(W_IN_SLABS):
            sl, sv = cx.load_slab(w_in[:, c0:c0 + C], 16, C)
            if si < 4:
                for i in range(NT):
                    b = cx.bank()
                    for k in range(16):
                        kb.op("pe", lambda h: h.matmul(b.t[:, 0:C], lhsT=xT[:, k, i * 128:(i + 1) * 128],
                                                       rhs=sv[:, k, :], start=(k == 0), stop=(k == 15)),
                              rd=[sl.R(), xT.R()], wr=[b.R()])
                    cx.copy(cx.evac_eng(), Hs[:, i, c0:c0 + C], b.t[:, 0:C], [b.R()], [Hs.R(i)])
            elif 10 <= si < 13:
                h0 = (si - 10) * 2
                for i in range(NT):
                    b = cx.bank()
                    for k in range(16):
                        kb.op("pe", lambda h: h.matmul(b.t[:, 0:256], lhsT=xT[:, k, i * 128:(i + 1) * 128],
                                                       rhs=sv[:, k, :], start=(k == 0), stop=(k == 15)),
                              rd=[sl.R(), xT.R()], wr=[b.R()])
                    cx.copy(cx.evac_eng(), vb[:, i, h0:h0 + 2, 0:128],
                            b.t[:, 0:256].rearrange("p (a d) -> p a d", d=128), [b.R()], [vb.R()])
            else:
                if si < 7:
                    dst, h0 = qbT, (si - 4) * 2
                elif si < 10:
                    dst, h0 = kbT, (si - 7) * 2
                else:
                    if si == 13:
                        qcT = fm.next()
                    dst, h0 = qcT, (si - 13) * 2
                for hh in range(2):
                    for tg in range(2):
                        b = cx.bank()
                        for k in range(16):
                            kb.op("pe", lambda h: h.matmul(b.t[:, :], lhsT=sv[:, k, hh * 128:(hh + 1) * 128],
                                                           rhs=xT[:, k, tg * 512:(tg + 1) * 512],
                                                           start=(k == 0), stop=(k == 15)),
                                  rd=[sl.R(), xT.R()], wr=[b.R()])
                        cx.copy(cx.evac_eng(), dst[:, h0 + hh, tg * 512:(tg + 1) * 512], b.t[:, :],
                                [b.R()], [dst.R()])
                if si == 6:
                    kb.op("sp", lambda h: h.dma_start(out=dq["qbT"].t[:], in_=qbT[:]), rd=[qbT.R()], wr=[dq["qbT"].R()], dma=True)
                if si == 14:
                    kb.op("sp", lambda h: h.dma_start(out=dq["qcT"].t[:], in_=qcT[:, 0:4, :]), rd=[qcT.R()], wr=[dq["qcT"].R()], dma=True)
            if si == 12:
                kb.op("sp", lambda h: h.dma_start(out=dk["vb"].t[:], in_=vb[:].rearrange("p i h d -> p i (h d)")),
                      rd=[vb.R()], wr=[dk["vb"].R()], dma=True)
        kb.op("sp", lambda h: h.dma_start(out=dk["kbT"].t[:], in_=kbT[:]), rd=[kbT.R()], wr=[dk["kbT"].R()], dma=True)
        for j in range(4):
            kb.op("dve", lambda h: h.tensor_reduce(out=kms[:, j, :], in_=kbT[:, :, j * 256:(j + 1) * 256],
                                                   axis=AX.X, op=ALU.add), rd=[kbT.R()], wr=[kms.R()])
        kb.op("dve", lambda h: h.tensor_scalar(out=kmb[:], in0=kms[:], scalar1=1.0 / 256, scalar2=None, op0=ALU.mult),
              rd=[kms.R()], wr=[kmb.R()])
        kb.op("sp", lambda h: h.dma_start(out=dk["kmT"].t[:], in_=kmb[:]), rd=[kmb.R()], wr=[dk["kmT"].R()], dma=True)

        kb.barrier()
        st1.close()
        cqnT = kb.sb(st, "cqnT", [128, 4, 1024], BF16)
        ckvnT = kb.sb(st, "ckvnT", [128, 2, 1024], BF16)
        kiT = kb.sb(st, "kiT", [64, 1024], BF16)
        wS = kb.sb(st, "wS", [128, 8, 16], F32)
        wN = kb.sb(st, "wN", [128, 8, 16], F32)
        wG = kb.sb(st, "wG", [128, 8, 16], F32)
        junk = kb.sb(st, "junk", [128, 512], F32)
        cqn = Ring([kb.sb(st, "cqn", [128, 768], BF16) for _ in range(2)])
        sm = Ring([kb.sb(st, "sm", [128, 16], F32) for _ in range(4)])
        kin = Ring([kb.sb(st, "kin", [128, 64], F32) for _ in range(2)])
        kinb = Ring([kb.sb(st, "kinb", [128, 64], BF16) for _ in range(2)])
        gq = cx.cst["g_cq"]; gkv = cx.cst["g_ckv"]; gki = cx.cst["g_kidx"]; bki = cx.cst["b_kidx"]
        for i in range(NT):
            s = sm.next()
            cq = cqn.next()
            for (c0, n, col, g) in ((0, 512, 0, gq), (512, 256, 1, gkv)):
                kb.op("act", lambda h: h.activation(out=junk[:, 0:n], in_=Hs[:, i, c0:c0 + n], func=AF.Square,
                                                    accum_out=s[:, col:col + 1]),
                      rd=[Hs.R(i)], wr=[junk.R(), s.R()])
                kb.op("act", lambda h: h.activation(out=s[:, 2 + col:3 + col], in_=s[:, col:col + 1], func=AF.Sqrt,
                                                    scale=1.0 / n, bias=cx.cst["epsc"][:, 0:1]),
                      rd=[s.R()], wr=[s.R()])
                kb.op("dve", lambda h: h.reciprocal(out=s[:, 4 + col:5 + col], in_=s[:, 2 + col:3 + col]),
                      rd=[s.R()], wr=[s.R()])
                kb.op("dve", lambda h: h.scalar_tensor_tensor(out=cq[:, c0:c0 + n], in0=Hs[:, i, c0:c0 + n],
                                                              scalar=s[:, 4 + col:5 + col], in1=g[:, l, :],
                                                              op0=ALU.mult, op1=ALU.mult),
                      rd=[Hs.R(i), s.R(), g.R()], wr=[cq.R()])
            cx.transpose_to(cq, [cq[:, c * 128:(c + 1) * 128] for c in range(4)],
                            lambda g0, n: cqnT[:, g0:g0 + n, i * 128:(i + 1) * 128], cqnT.R(), BF16)
            cx.transpose_to(cq, [cq[:, 512 + c * 128:512 + (c + 1) * 128] for c in range(2)],
                            lambda g0, n: ckvnT[:, g0:g0 + n, i * 128:(i + 1) * 128], ckvnT.R(), BF16)
            ki = kin.next(); kib = kinb.next()
            kb.op("dve", lambda h: h.tensor_reduce(out=s[:, 6:7], in_=Hs[:, i, 768:832], axis=AX.X, op=ALU.add),
                  rd=[Hs.R(i)], wr=[s.R()])
            kb.op("dve", lambda h: h.tensor_scalar(out=s[:, 7:8], in0=s[:, 6:7], scalar1=-1.0 / 64, scalar2=None,
                                                   op0=ALU.mult), rd=[s.R()], wr=[s.R()])
            kb.op("dve", lambda h: h.tensor_scalar(out=ki[:], in0=Hs[:, i, 768:832], scalar1=s[:, 7:8], scalar2=None,
                                                   op0=ALU.add), rd=[Hs.R(i), s.R()], wr=[ki.R()])
            kb.op("act", lambda h: h.activation(out=junk[:, 0:64], in_=ki[:], func=AF.Square, accum_out=s[:, 8:9]),
                  rd=[ki.R()], wr=[junk.R(), s.R()])
            kb.op("act", lambda h: h.activation(out=s[:, 9:10], in_=s[:, 8:9], func=AF.Sqrt, scale=1.0 / 64,
                                                bias=cx.cst["epsc"][:, 0:1]), rd=[s.R()], wr=[s.R()])
            kb.op("dve", lambda h: h.reciprocal(out=s[:, 10:11], in_=s[:, 9:10]), rd=[s.R()], wr=[s.R()])
            kb.op("dve", lambda h: h.scalar_tensor_tensor(out=ki[:], in0=ki[:], scalar=s[:, 10:11], in1=gki[:, l, :],
                                                          op0=ALU.mult, op1=ALU.mult),
                  rd=[ki.R(), s.R(), gki.R()], wr=[ki.R()])
            kb.op("dve", lambda h: h.tensor_tensor(out=kib[:], in0=ki[:], in1=bki[:, l, :], op=ALU.add),
                  rd=[ki.R(), bki.R()], wr=[kib.R()])
            cx.transpose_to(kib, [kib[:, 0:64]],
                            lambda g0, n: kiT[0:64, i * 128:(i + 1) * 128].rearrange("p (n t) -> p n t", n=1),
                            kiT.R(), BF16, rows=64)
            kb.op("dve", lambda h: h.tensor_scalar(out=wS[:, i, :], in0=Hs[:, i, 832:848], scalar1=1.0 / 32, scalar2=None,
                                                   op0=ALU.mult), rd=[Hs.R(i)], wr=[wS.R(i)])
            kb.op("dve", lambda h: h.tensor_scalar(out=wN[:, i, :], in0=wS[:, i, :], scalar1=0.0, scalar2=None,
                                                   op0=ALU.min), rd=[wS.R(i)], wr=[wN.R(i)])
            kb.op("dve", lambda h: h.tensor_scalar(out=wG[:, i, :], in0=wS[:, i, :], scalar1=0.0, scalar2=2.0,
                                                   op0=ALU.is_ge, op1=ALU.mult), rd=[wS.R(i)], wr=[wG.R()])
            kb.op("dve", lambda h: h.tensor_scalar(out=wG[:, i, :], in0=wG[:, i, :], scalar1=-1.0, scalar2=None,
                                                   op0=ALU.add), rd=[wG.R()], wr=[wG.R()])
        kb.op("sp", lambda h: h.dma_start(out=dk["kiT"].t[:], in_=kiT[:]), rd=[kiT.R()], wr=[dk["kiT"].R()], dma=True)
        kb.op("sp", lambda h: h.dma_start(out=dq["wsg"].t[:], in_=wG[:]), rd=[wG.R()], wr=[dq["wsg"].R()], dma=True)

        qiT = kb.sb(st, "qiT", [128, 9, 1024], BF16)
        qs = Ring([kb.sb(st, "qs", [128, 18, 64], BF16) for _ in range(2)])
        qtmp = kb.sb(st, "qtmp", [128, 16, 64], F32)
        q17 = kb.sb(st, "q17", [128, 64], F32)
        sl_i, sv_i = cx.load_slab(din["w_uqi"].t[0], 4, 1024)
        for i in range(NT):
            q = qs.next()
            if i < 2:
                kb.op("pool", lambda h: h.memset(q[:, 17, :], 0.0), wr=[q.R()])
            b0 = cx.bank(); b1 = cx.bank()
            for half, b in ((0, b0), (1, b1)):
                for k in range(4):
                    kb.op("pe", lambda h: h.matmul(b.t[:, :], lhsT=cqnT[:, k, i * 128:(i + 1) * 128],
                                                   rhs=sv_i[:, k, half * 512:(half + 1) * 512],
                                                   start=(k == 0), stop=(k == 3)),
                          rd=[sl_i.R(), cqnT.R()], wr=[b.R()])
                bv = b.t[:, :].rearrange("p (a d) -> p a d", d=64)
                kb.op("dve", lambda h: h.tensor_tensor(out=q[:, half * 8:(half + 1) * 8, :], in0=bv,
                                                       in1=wS[:, i, half * 8:(half + 1) * 8].unsqueeze(2).to_broadcast([128, 8, 64]),
                                                       op=ALU.mult), rd=[b.R(), wS.R(i)], wr=[q.R()])
                kb.op("dve", lambda h: h.tensor_tensor(out=qtmp[:, half * 8:(half + 1) * 8, :], in0=bv,
                                                       in1=wN[:, i, half * 8:(half + 1) * 8].unsqueeze(2).to_broadcast([128, 8, 64]),
                                                       op=ALU.mult), rd=[b.R(), wN.R(i)], wr=[qtmp.R()])
            kb.op("dve", lambda h: h.tensor_reduce(out=q17[:], in_=qtmp[:].rearrange("p a d -> p d a"), axis=AX.X,
                                                   op=ALU.add), rd=[qtmp.R()], wr=[q17.R()])
            kb.op("dve", lambda h: h.tensor_copy(out=q[:, 16, :], in_=q17[:]), rd=[q17.R()], wr=[q.R()])
            qf = q[:].rearrange("p a d -> p (a d)")
            cx.transpose_to(q, [qf[:, c * 128:(c + 1) * 128] for c in range(9)],
                            lambda g0, n: qiT[:, g0:g0 + n, i * 128:(i + 1) * 128], qiT.R(), BF16)
        kb.op("sp", lambda h: h.dma_start(out=dq["qiT"].t[:], in_=qiT[:]), rd=[qiT.R()], wr=[dq["qiT"].R()], dma=True)

        qaT = fm.next(); kaT = fm.next()
        sl_q, sv_q = cx.load_slab(din["w_uq"].t[0], 4, 768)
        for hh in range(6):
            for tg in range(2):
                b = cx.bank()
                for k in range(4):
                    kb.op("pe", lambda h: h.matmul(b.t[:, :], lhsT=sv_q[:, k, hh * 128:(hh + 1) * 128],
                                                   rhs=cqnT[:, k, tg * 512:(tg + 1) * 512], start=(k == 0), stop=(k == 3)),
                          rd=[sl_q.R(), cqnT.R()], wr=[b.R()])
                cx.copy(cx.evac_eng(), qaT[:, hh, tg * 512:(tg + 1) * 512], b.t[:, :], [b.R()], [qaT.R()])
        kb.op("sp", lambda h: h.dma_start(out=dq["qaT"].t[:], in_=qaT[:]), rd=[qaT.R()], wr=[dq["qaT"].R()], dma=True)
        sl_k, sv_k = cx.load_slab(din["w_ukv"].t[0], 2, 1536)
        for hh in range(6):
            for tg in range(2):
                b = cx.bank()
                for k in range(2):
                    kb.op("pe", lambda h: h.matmul(b.t[:, :], lhsT=sv_k[:, k, hh * 128:(hh + 1) * 128],
                                                   rhs=ckvnT[:, k, tg * 512:(tg + 1) * 512], start=(k == 0), stop=(k == 1)),
                          rd=[sl_k.R(), ckvnT.R()], wr=[b.R()])
                cx.copy(cx.evac_eng(), kaT[:, hh, tg * 512:(tg + 1) * 512], b.t[:, :], [b.R()], [kaT.R()])
        kb.op("sp", lambda h: h.dma_start(out=dk["kaT"].t[:], in_=kaT[:]), rd=[kaT.R()], wr=[dk["kaT"].R()], dma=True)
        for i in range(NT):
            for part, (c0, n) in enumerate(((768, 512), (1280, 256))):
                b = cx.bank()
                for k in range(2):
                    kb.op("pe", lambda h: h.matmul(b.t[:, 0:n], lhsT=ckvnT[:, k, i * 128:(i + 1) * 128],
                                                   rhs=sv_k[:, k, c0:c0 + n], start=(k == 0), stop=(k == 1)),
                          rd=[sl_k.R(), ckvnT.R()], wr=[b.R()])
                h0 = part * 4
                nh = n // 128
                cx.copy(cx.evac_eng(), va[:, i, h0:h0 + nh, 0:128],
                        b.t[:, 0:n].rearrange("p (a d) -> p a d", d=128), [b.R()], [va.R()])
        kb.op("sp", lambda h: h.dma_start(out=dk["va"].t[:], in_=va[:].rearrange("p i h d -> p i (h d)")),
              rd=[va.R()], wr=[dk["va"].R()], dma=True)
    kb.barrier()


def kq_unit(q):
    j, r = q // 8, q % 8
    return r, j, unit_of(r, j)


def phase_B(cx, l, dq, dka, d_attn, mkT, mv):
    kb = cx.kb
    C = cx.cst
    with ExitStack() as st:
        def view_bf16(tile):
            v = Tile(tile.t[:].bitcast(BF16))
            v.res = tile.res
            return v
        kiT2 = view_bf16(cx.stage.tiles[0])
        maskts = view_bf16(cx.stage.tiles[1])
        score = kb.sb(st, "score", [128, 8192], F32)
        maskT = kb.sb(st, "maskT", [128, 64, 256], BF16)
        qi = kb.sb(st, "qi", [128, 9, 256], BF16)
        qa = kb.sb(st, "qa", [128, 6, 256], BF16)
        qb = kb.sb(st, "qb", [128, 6, 256], BF16)
        qc = kb.sb(st, "qc", [128, 4, 256], BF16)
        kmT = kb.sb(st, "kmTa", [128, 8, 4, 6], BF16)
        kring = Ring([kb.sb(st, "ku", [128, 6, 256], BF16) for _ in range(2)])
        vring = Ring([kb.sb(st, "vu", [128, 2, 774], BF16) for _ in range(2)])
        pring = Ring([kb.sb(st, "pT", [128, 128], BF16) for _ in range(4)])
        p2ring = Ring([kb.sb(st, "pT2", [128, 128], BF16) for _ in range(4)])
        cmring = Ring([kb.sb(st, "cm", [128, 256], BF16) for _ in range(2)])
        accO = kb.sb(st, "accO", [128, 12, 129], F32)
        onrm = Ring([kb.sb(st, "onrm", [128, 128], BF16) for _ in range(3)])
        attn_s = kb.sb(st, "attn_s", [128, 16, 256], BF16)
        small = Ring([kb.sb(st, "smB", [128, 16], F32) for _ in range(6)])
        bs = kb.sb(st, "bs", [128, 8], F32)
        gate = kb.sb(st, "gate", [128, 6, 32], F32)
        wsel = kb.sb(st, "wsel", [128, 2, 6, 32], F32)
        vbias = kb.sb(st, "vbias", [128, 32], F32)
        own = kb.sb(st, "own", [128, 32], F32)
        top8 = kb.sb(st, "top8", [128, 8], F32)
        tmp32 = kb.sb(st, "tmp32", [128, 32], F32)
        mb = Ring([kb.sb(st, "mb", [128, 256], F32) for _ in range(2)])
        Mq = kb.sb(st, "Mq", [128, 32], F32)
        sg = kb.sb(st, "sg", [128, 2, 16], F32)
        rlr = Ring([kb.sb(st, "rl", [128, 512], F32) for _ in range(3)])
        pref = kb.sb(st, "pref", [128, 4], F32)
        w6r = Ring([kb.sb(st, "w6", [128, 6], F32) for _ in range(10)])
        e6r = Ring([kb.sb(st, "e6", [128, 6], F32) for _ in range(4)])

        kb.op("sp", lambda h: h.dma_start(out=kmT[:], in_=dka["kmT"].t[:].rearrange("r p j h -> p r j h")),
              rd=[dka["kmT"].R()], wr=[kmT.R()], dma=True)

        for j in range(4):
            NB = 8 * (j + 1)
            NK = NB * 256
            tsl = slice(j * 256, (j + 1) * 256)
            for nm, t_, d_ in (("qiT", qi, dq["qiT"]), ("qaT", qa, dq["qaT"]), ("qbT", qb, dq["qbT"]), ("qcT", qc, dq["qcT"])):
                kb.op("sp", lambda h: h.dma_start(out=t_[:], in_=d_.t[:, :, tsl]), rd=[d_.R()], wr=[t_.R()], dma=True)
            kb.op("sp", lambda h: h.dma_start(out=sg[:], in_=dq["wsg"].t[:, 2 * j:2 * j + 2, :]), rd=[dq["wsg"].R()], wr=[sg.R()], dma=True)
            if True:
                for half in range(2):
                    kb.op("sp", lambda h: h.dma_start(
                        out=kiT2[half * 64:(half + 1) * 64, j * 2048:(j + 1) * 2048].rearrange("p (r s) -> p r s", s=256),
                        in_=dka["kiT"].t[:, :, tsl].rearrange("r p s -> p r s")),
                        rd=[dka["kiT"].R()], wr=[kiT2.R()], dma=True)
            for qt in range(2):
                i = 2 * j + qt
                for c in range(NK // 512):
                    for hh in range(16):
                        b = cx.bank()
                        po = (hh % 2) * 64
                        kb.op("pe", lambda h: h.matmul(b.t[:, :], lhsT=qi[po:po + 64, hh // 2, qt * 128:(qt + 1) * 128],
                                                       rhs=kiT2[po:po + 64, c * 512:(c + 1) * 512], start=True, stop=True),
                              rd=[qi.R(), kiT2.R()], wr=[b.R()])
                        sc = score[:, c * 512:(c + 1) * 512]
                        rl = rlr.next()
                        sgc = sg[:, qt, hh:hh + 1]
                        kb.op("act", lambda h: h.activation(out=rl[:], in_=b.t[:, :], func=AF.Relu, scale=sgc),
                              rd=[b.R(), sg.R()], wr=[rl.R()])
                        if hh == 0:
                            kb.op("dve", lambda h: h.tensor_scalar(out=sc, in0=rl[:], scalar1=sgc, scalar2=None, op0=ALU.mult),
                                  rd=[rl.R(), sg.R()], wr=[score.R()])
                        else:
                            kb.op("dve", lambda h: h.scalar_tensor_tensor(out=sc, in0=rl[:], scalar=sgc, in1=sc,
                                                                          op0=ALU.mult, op1=ALU.add),
                                  rd=[rl.R(), sg.R(), score.R()], wr=[score.R()])
                kb.op("dve", lambda h: h.tensor_reduce(out=bs[:, 5:6], in_=score[:, 0:NK], axis=AX.X, op=ALU.max),
                      rd=[score.R()], wr=[bs.R()])
                kb.op("dve", lambda h: h.tensor_reduce(out=bs[:, 6:7], in_=score[:, 0:NK], axis=AX.X, op=ALU.min),
                      rd=[score.R()], wr=[bs.R()])
                kb.op("dve", lambda h: h.scalar_tensor_tensor(out=bs[:, 5:6], in0=bs[:, 6:7], scalar=-1.0, in1=bs[:, 5:6],
                                                              op0=ALU.mult, op1=ALU.max), rd=[bs.R()], wr=[bs.R()])
                kb.op("dve", lambda h: h.tensor_scalar(out=bs[:, 5:6], in0=bs[:, 5:6], scalar1=1.001, scalar2=1e-6,
                                                       op0=ALU.mult, op1=ALU.add), rd=[bs.R()], wr=[bs.R()])
                kb.op("dve", lambda h: h.tensor_scalar(out=bs[:, 0:1], in0=bs[:, 5:6], scalar1=-1.0, scalar2=None, op0=ALU.mult),
                      rd=[bs.R()], wr=[bs.R()])
                kb.op("dve", lambda h: h.tensor_scalar(out=bs[:, 1:2], in0=bs[:, 5:6], scalar1=2.0, scalar2=None, op0=ALU.mult),
                      rd=[bs.R()], wr=[bs.R()])
                for r in range(8):
                    q = 8 * j + r
                    u = unit_of(r, j)
                    m = mb.next()
                    sm_ = small.next()
                    kb.op("dve", lambda h: h.tensor_scalar(out=sm_[:, 0:1], in0=C["pos_col"][:, i:i + 1], scalar1=float(-256 * u),
                                                           scalar2=None, op0=ALU.add), rd=[C["pos_col"].R()], wr=[sm_.R()])
                    kb.op("dve", lambda h: h.tensor_scalar(out=m[:], in0=C["iota256"][:], scalar1=sm_[:, 0:1], scalar2=NEG,
                                                           op0=ALU.is_gt, op1=ALU.mult), rd=[C["iota256"].R(), sm_.R()], wr=[m.R()])
                    kb.op("dve", lambda h: h.tensor_tensor(out=score[:, q * 256:(q + 1) * 256], in0=score[:, q * 256:(q + 1) * 256],
                                                           in1=m[:], op=ALU.add), rd=[m.R(), score.R()], wr=[score.R()])
                for it in range(N_BISECT):
                    ck = 0.5 ** (it + 1)
                    kb.op("dve", lambda h: h.scalar_tensor_tensor(out=bs[:, 2:3], in0=bs[:, 1:2], scalar=ck, in1=bs[:, 0:1],
                                                                  op0=ALU.mult, op1=ALU.add), rd=[bs.R()], wr=[bs.R()])
                    kb.op("dve", lambda h: h.tensor_scalar(out=maskts[:, 0:NK], in0=score[:, 0:NK], scalar1=bs[:, 2:3], scalar2=None,
                                                           op0=ALU.is_gt, op1=ALU.add, accum_out=bs[:, 3:4]),
                          rd=[score.R(), bs.R()], wr=[maskts.R(), bs.R()])
                    kb.op("dve", lambda h: h.tensor_scalar(out=bs[:, 4:5], in0=bs[:, 3:4], scalar1=255.5, scalar2=bs[:, 1:2],
                                                           op0=ALU.is_ge, op1=ALU.mult), rd=[bs.R()], wr=[bs.R()])
                    kb.op("dve", lambda h: h.scalar_tensor_tensor(out=bs[:, 0:1], in0=bs[:, 4:5], scalar=ck, in1=bs[:, 0:1],
                                                                  op0=ALU.mult, op1=ALU.add), rd=[bs.R()], wr=[bs.R()])
                kb.op("dve", lambda h: h.tensor_scalar(out=maskts[:, 0:NK], in0=score[:, 0:NK], scalar1=bs[:, 0:1], scalar2=None,
                                                       op0=ALU.is_gt), rd=[score.R(), bs.R()], wr=[maskts.R()])
                cx.transpose_to(maskts, [maskts[:, kt * 128:(kt + 1) * 128] for kt in range(NB * 2)],
                                lambda g0, n: maskT[:, g0:g0 + n, qt * 128:(qt + 1) * 128], maskT.R(), BF16)
                for q in range(NB):
                    m = mb.next()
                    kb.op("dve", lambda h: h.scalar_tensor_tensor(out=m[:], in0=score[:, q * 256:(q + 1) * 256], scalar=bs[:, 0:1],
                                                                  in1=C["iota256p1"][:], op0=ALU.is_gt, op1=ALU.mult),
                          rd=[score.R(), bs.R(), C["iota256p1"].R()], wr=[m.R()])
                    kb.op("dve", lambda h: h.tensor_reduce(out=Mq[:, q:q + 1], in_=m[:], axis=AX.X, op=ALU.max), rd=[m.R()], wr=[Mq.R()])
                kb.op("dve", lambda h: h.tensor_scalar(out=tmp32[:, 0:NB], in0=Mq[:, 0:NB], scalar1=0.5, scalar2=None, op0=ALU.is_gt),
                      rd=[Mq.R()], wr=[tmp32.R()])
                kb.op("dve", lambda h: h.tensor_tensor(out=tmp32[:, 0:NB], in0=tmp32[:, 0:NB], in1=C["ucol"][:, 0:NB], op=ALU.mult),
                      rd=[tmp32.R(), C["ucol"].R()], wr=[tmp32.R()])
                kb.op("dve", lambda h: h.tensor_tensor(out=tmp32[:, 0:NB], in0=tmp32[:, 0:NB], in1=Mq[:, 0:NB], op=ALU.add),
                      rd=[tmp32.R(), Mq.R()], wr=[tmp32.R()])
                kb.op("dve", lambda h: h.tensor_reduce(out=pref[:, qt:qt + 1], in_=tmp32[:, 0:NB], axis=AX.X, op=ALU.max),
                      rd=[tmp32.R()], wr=[pref.R()])
                kb.op("dve", lambda h: h.tensor_scalar(out=pref[:, qt:qt + 1], in0=pref[:, qt:qt + 1], scalar1=-1.0, scalar2=None, op0=ALU.add),
                      rd=[pref.R()], wr=[pref.R()])

            kb.op("dve", lambda h: h.tensor_scalar(out=vbias[:], in0=C["ucol"][:], scalar1=C["ustart"][:, j:j + 1], scalar2=NEG,
                                                   op0=ALU.is_ge, op1=ALU.mult), rd=[C["ucol"].R(), C["ustart"].R()], wr=[vbias.R()])
            kb.op("dve", lambda h: h.tensor_scalar(out=own[:], in0=C["ucol"][:], scalar1=C["ustart"][:, j:j + 1], scalar2=None,
                                                   op0=ALU.is_equal), rd=[C["ucol"].R(), C["ustart"].R()], wr=[own.R()])
            for qt in range(2):
                b = cx.bank()
                for hh in range(6):
                    kb.op("pe", lambda h: h.matmul(b.t[:, hh * 32:(hh + 1) * 32], lhsT=qb[:, hh, qt * 128:(qt + 1) * 128],
                                                   rhs=kmT[:, :, :, hh].rearrange("p r j -> p j r"), start=True, stop=True),
                          rd=[qb.R(), kmT.R()], wr=[b.R()])
                kb.op("dve", lambda h: h.tensor_tensor(out=gate[:], in0=b.t[:, 0:192].rearrange("p (a n) -> p a n", n=32),
                                                       in1=vbias[:].unsqueeze(1).to_broadcast([128, 6, 32]), op=ALU.add),
                      rd=[b.R(), vbias.R()], wr=[gate.R()])
                for hh in range(6):
                    kb.op("dve", lambda h: h.max(out=top8[:], in_=gate[:, hh, :]), rd=[gate.R()], wr=[top8.R()])
                    kb.op("dve", lambda h: h.tensor_scalar(out=tmp32[:], in0=gate[:, hh, :], scalar1=top8[:, 2:3], scalar2=None,
                                                           op0=ALU.is_ge), rd=[gate.R(), top8.R()], wr=[tmp32.R()])
                    kb.op("dve", lambda h: h.scalar_tensor_tensor(out=tmp32[:], in0=gate[:, hh, :], scalar=-1e29, in1=tmp32[:],
                                                                  op0=ALU.is_gt, op1=ALU.mult), rd=[gate.R(), tmp32.R()], wr=[tmp32.R()])
                    kb.op("dve", lambda h: h.tensor_tensor(out=wsel[:, qt, hh, :], in0=tmp32[:], in1=own[:], op=ALU.add),
                          rd=[tmp32.R(), own.R()], wr=[wsel.R()])

            for qt in range(2):
                kb.op("dve", lambda h: h.tensor_copy(out=pref[:, 2 + qt:3 + qt], in_=C["pos_col"][:, 2 * j + qt:2 * j + qt + 1]),
                      rd=[C["pos_col"].R()], wr=[pref.R()])
            for kind in range(2):
                kname, vname = ("kaT", "va") if kind == 0 else ("kbT", "vb")
                qT = qa if kind == 0 else qb
                kb.op("pool", lambda h: h.memset(accO[:], 0.0), wr=[accO.R()])
                for q in range(NB):
                    r, jj, u = kq_unit(q)
                    band = (jj == j)
                    ku = kring.next(); vu = vring.next()
                    kb.op("sp", lambda h: h.dma_start(out=ku[:], in_=dka[kname].t[r, :, :, jj * 256:(jj + 1) * 256]),
                          rd=[dka[kname].R()], wr=[ku.R()], dma=True)
                    kb.op("sp", lambda h: h.dma_start(out=vu[:], in_=dka[vname].t[r, :, 2 * jj:2 * jj + 2, :]),
                          rd=[dka[vname].R()], wr=[vu.R()], dma=True)
                    vu4 = vu[:].rearrange("p k (a d) -> p k a d", d=129)
                    wl = {}
                    cml = {}
                    for kt in range(2):
                        for qt in range(2):
                            sm_ = small.next(); e6 = e6r.next(); w6 = w6r.next()
                            ckt = float(256 * u + 128 * kt + 127)
                            pc = kind * 2 + qt
                            kb.op("pool", lambda h: h.tensor_scalar(out=sm_[:, 0:1], in0=pref[:, pc:pc + 1], scalar1=-1.0, scalar2=ckt,
                                                                    op0=ALU.mult, op1=ALU.add), rd=[pref.R()], wr=[sm_.R()])
                            kb.op("pool", lambda h: h.tensor_scalar(out=e6[:], in0=C["slopes"][:, kind * 6:kind * 6 + 6], scalar1=sm_[:, 0:1],
                                                                    scalar2=80.0, op0=ALU.mult, op1=ALU.min),
                                  rd=[C["slopes"].R(), sm_.R()], wr=[e6.R()])
                            kb.op("act", lambda h: h.activation(out=w6[:], in_=e6[:], func=AF.Exp), rd=[e6.R()], wr=[w6.R()])
                            if kind == 1:
                                qcol = jj * 8 + r
                                kb.op("pool", lambda h: h.tensor_tensor(out=w6[:], in0=w6[:], in1=wsel[:, qt, :, qcol], op=ALU.mult),
                                      rd=[w6.R(), wsel.R()], wr=[w6.R()])
                            wl[(kt, qt)] = w6
                        if band and kind == 1:
                            cm = cmring.next(); sm2 = small.next()
                            kb.op("pool", lambda h: h.tensor_scalar(out=sm2[:, 0:1], in0=C["iota_p"][:, 0:1],
                                                                    scalar1=float(256 * u + 128 * kt), scalar2=None, op0=ALU.add),
                                  rd=[C["iota_p"].R()], wr=[sm2.R()])
                            kb.op("pool", lambda h: h.tensor_scalar(out=cm[:], in0=C["pos_row"][:, tsl], scalar1=sm2[:, 0:1], scalar2=None,
                                                                    op0=ALU.is_ge), rd=[C["pos_row"].R(), sm2.R()], wr=[cm.R()])
                            cml[kt] = cm
                    for hh in range(6):
                        ob = [cx.bank(), cx.bank()]
                        for kt in range(2):
                            b = cx.bank()
                            kb.op("pe", lambda h: h.matmul(b.t[:, 0:256], lhsT=ku[:, hh, kt * 128:(kt + 1) * 128], rhs=qT[:, hh, :],
                                                           start=True, stop=True), rd=[ku.R(), qT.R()], wr=[b.R()])
                            for qt in range(2):
                                p = pring.next()
                                kb.op("act", lambda h: h.activation(out=p[:], in_=b.t[:, qt * 128:(qt + 1) * 128], func=AF.Exp,
                                                                    scale=SCALE, bias=C["abias"][:, kind * 6 + hh:kind * 6 + hh + 1]),
                                      rd=[b.R(), C["abias"].R()], wr=[p.R()])
                                p2 = p
                                if kind == 0:
                                    p2 = p2ring.next()
                                    kb.op("pool", lambda h: h.tensor_tensor(out=p2[:], in0=p[:], in1=maskT[:, 2 * q + kt, qt * 128:(qt + 1) * 128],
                                                                            op=ALU.mult), rd=[p.R(), maskT.R()], wr=[p2.R()])
                                elif band:
                                    p2 = p2ring.next()
                                    kb.op("pool", lambda h: h.tensor_tensor(out=p2[:], in0=p[:], in1=cml[kt][:, qt * 128:(qt + 1) * 128],
                                                                            op=ALU.mult), rd=[p.R(), cml[kt].R()], wr=[p2.R()])
                                kb.op("pe", lambda h: h.matmul(ob[kt].t[:, qt * 256:qt * 256 + 129], lhsT=p2[:], rhs=vu4[:, kt, hh, :],
                                                               start=True, stop=True), rd=[p2.R(), vu.R()], wr=[ob[kt].R()])
                        for kt in range(2):
                            for qt in range(2):
                                a_ = accO[:, hh * 2 + qt, :]
                                kb.op("dve", lambda h: h.scalar_tensor_tensor(out=a_, in0=ob[kt].t[:, qt * 256:qt * 256 + 129],
                                                                              scalar=wl[(kt, qt)][:, hh:hh + 1], in1=a_, op0=ALU.mult, op1=ALU.add),
                                      rd=[ob[kt].R(), accO.R(), wl[(kt, qt)].R()], wr=[accO.R()])
                for hh in range(6):
                    for qt in range(2):
                        sm_ = small.next(); o_ = onrm.next()
                        a_ = accO[:, hh * 2 + qt, :]
                        kb.op("dve", lambda h: h.reciprocal(out=sm_[:, 0:1], in_=a_[:, 128:129]), rd=[accO.R()], wr=[sm_.R()])
                        kb.op("dve", lambda h: h.tensor_scalar(out=o_[:], in0=a_[:, 0:128], scalar1=sm_[:, 0:1], scalar2=None, op0=ALU.mult),
                              rd=[accO.R(), sm_.R()], wr=[o_.R()])
                        cx.transpose_to(o_, [o_[:]], lambda g0, n: attn_s[:, kind * 6 + hh, qt * 128:(qt + 1) * 128].rearrange("p (n t) -> p n t", n=1),
                                        attn_s.R(), BF16)
            mv4 = mv
            for hc in range(4):
                ob = [cx.bank(), cx.bank()]
                for mt in range(2):
                    b = cx.bank()
                    kb.op("pe", lambda h: h.matmul(b.t[:, 0:256], lhsT=mkT[:, hc, mt * 128:(mt + 1) * 128], rhs=qc[:, hc, :],
                                                   start=True, stop=True), rd=[mkT.R(), qc.R()], wr=[b.R()])
                    for qt in range(2):
                        p = pring.next()
                        kb.op("act", lambda h: h.activation(out=p[:], in_=b.t[:, qt * 128:(qt + 1) * 128], func=AF.Exp, scale=SCALE),
                              rd=[b.R()], wr=[p.R()])
                        kb.op("pe", lambda h: h.matmul(ob[qt].t[:, 0:129], lhsT=p[:], rhs=mv4[:, mt, hc, :],
                                                       start=(mt == 0), stop=(mt == 1)), rd=[p.R(), mv.R()], wr=[ob[qt].R()])
                for qt in range(2):
                    sm_ = small.next(); o_ = onrm.next()
                    kb.op("dve", lambda h: h.reciprocal(out=sm_[:, 0:1], in_=ob[qt].t[:, 128:129]), rd=[ob[qt].R()], wr=[sm_.R()])
                    kb.op("dve", lambda h: h.tensor_scalar(out=o_[:], in0=ob[qt].t[:, 0:128], scalar1=sm_[:, 0:1], scalar2=None, op0=ALU.mult),
                          rd=[ob[qt].R(), sm_.R()], wr=[o_.R()])
                    cx.transpose_to(o_, [o_[:]], lambda g0, n: attn_s[:, 12 + hc, qt * 128:(qt + 1) * 128].rearrange("p (n t) -> p n t", n=1),
                                    attn_s.R(), BF16)
            kb.op("sp", lambda h: h.dma_start(out=d_attn.t[:, :, tsl], in_=attn_s[:]), rd=[attn_s.R()], wr=[d_attn.R()], dma=True)
    kb.barrier()


def layer_norm_tile(cx, x_ap, x_res, gb, gb_res, sm_, junk):
    kb = cx.kb
    kb.op("dve", lambda h: h.tensor_reduce(out=sm_[:, 0:1], in_=x_ap, axis=AX.X, op=ALU.add), rd=[x_res], wr=[sm_.R()])
    kb.op("dve", lambda h: h.tensor_scalar(out=sm_[:, 1:2], in0=sm_[:, 0:1], scalar1=-1.0 / 2048, scalar2=None, op0=ALU.mult),
          rd=[sm_.R()], wr=[sm_.R()])
    kb.op("dve", lambda h: h.tensor_scalar(out=x_ap, in0=x_ap, scalar1=sm_[:, 1:2], scalar2=None, op0=ALU.add),
          rd=[x_res, sm_.R()], wr=[x_res])
    kb.op("act", lambda h: h.activation(out=junk[:], in_=x_ap, func=AF.Square, accum_out=sm_[:, 2:3]),
          rd=[x_res], wr=[junk.R(), sm_.R()])
    kb.op("act", lambda h: h.activation(out=sm_[:, 3:4], in_=sm_[:, 2:3], func=AF.Sqrt, scale=1.0 / 2048,
                                        bias=cx.cst["epsc"][:, 0:1]), rd=[sm_.R()], wr=[sm_.R()])
    kb.op("dve", lambda h: h.reciprocal(out=sm_[:, 4:5], in_=sm_[:, 3:4]), rd=[sm_.R()], wr=[sm_.R()])
    kb.op("dve", lambda h: h.scalar_tensor_tensor(out=x_ap, in0=x_ap, scalar=sm_[:, 4:5], in1=gb[:, 0:2048],
                                                  op0=ALU.mult, op1=ALU.mult), rd=[x_res, sm_.R(), gb_res], wr=[x_res])
    kb.op("pool", lambda h: h.tensor_tensor(out=x_ap, in0=x_ap, in1=gb[:, 2048:4096], op=ALU.add),
          rd=[x_res, gb_res], wr=[x_res])


def load_gb(cx, dg, db, l):
    kb = cx.kb
    gb = cx.stage.next()
    kb.op("sp", lambda h: h.dma_start(out=gb[:, 0:2048], in_=dg.t[:, l, :]), wr=[gb.R()], dma=True)
    kb.op("sp", lambda h: h.dma_start(out=gb[:, 2048:4096], in_=db.t[:, l, :]), wr=[gb.R()], dma=True)
    return gb


def phase_C(cx, l, xs_d, d_attn, din, d_x1pre, d_out):
    kb = cx.kb
    C = cx.cst
    with ExitStack() as st:
        smr = Ring([kb.sb(st, "smC", [128, 16], F32) for _ in range(4)])
        with ExitStack() as s1:
            xT = kb.sb(s1, "xTc", [128, 16, 1024], BF16)
            attnT = kb.sb(s1, "attnT", [128, 16, 1024], BF16)
            mergedT = kb.sb(s1, "mergedT", [128, 16, 1024], BF16)
            gtr = Ring([kb.sb(s1, "gts", [128, 2, 1024], BF16) for _ in range(2)])
            macc = kb.sb(s1, "macc", [128, 4, 512], F32)
            tmpr = Ring([kb.sb(s1, "tmpm", [128, 512], F32) for _ in range(1)])
            xblk = Ring([kb.sb(s1, "xblk", [128, 256], F32) for _ in range(2)])
            kb.op("sp", lambda h: h.dma_start(out=attnT[:], in_=d_attn.t[:]), rd=[d_attn.R()], wr=[attnT.R()], dma=True)
            for i in range(NT):
                xi = cx.stage.next()
                kb.op("sp", lambda h: h.dma_start(out=xi[:, 0:2048], in_=xs_d.t[i * 128:(i + 1) * 128, :]),
                      rd=[xs_d.R()], wr=[xi.R()], dma=True)
                cx.transpose_to(xi, [xi[:, c * 128:(c + 1) * 128] for c in range(16)],
                                lambda g0, n: xT[:, g0:g0 + n, i * 128:(i + 1) * 128], xT.R(), F32)
            w_gate = din["w_gate"].t[0]
            ups = ((din["w_up_a"].t[0], 6, 0), (din["w_up_b"].t[0], 6, 6), (din["w_up_c"].t[0], 4, 12))
            for fp in range(8):
                for gi, (wu, nk, off) in enumerate(ups):
                    gt = gtr.next()
                    sl, sv = cx.load_slab(w_gate[:, gi * 2048 + fp * 256: gi * 2048 + (fp + 1) * 256], 16, 256)
                    for fc2 in range(2):
                        for tg in range(2):
                            b = cx.bank()
                            for k in range(16):
                                kb.op("pe", lambda h: h.matmul(b.t[:, :], lhsT=sv[:, k, fc2 * 128:(fc2 + 1) * 128],
                                                               rhs=xT[:, k, tg * 512:(tg + 1) * 512], start=(k == 0), stop=(k == 15)),
                                      rd=[sl.R(), xT.R()], wr=[b.R()])
                            col = gi * 16 + fp * 2 + fc2
                            kb.op("act", lambda h: h.activation(out=gt[:, fc2, tg * 512:(tg + 1) * 512], in_=b.t[:, :],
                                                                func=AF.Sigmoid, bias=C["b_gate"][:, l, col:col + 1]),
                                  rd=[b.R(), C["b_gate"].R()], wr=[gt.R()])
                    sl, sv = cx.load_slab(wu[:, fp * 256:(fp + 1) * 256], nk, 256)
                    for fc2 in range(2):
                        for tg in range(2):
                            b = cx.bank()
                            for k in range(nk):
                                kb.op("pe", lambda h: h.matmul(b.t[:, :], lhsT=sv[:, k, fc2 * 128:(fc2 + 1) * 128],
                                                               rhs=attnT[:, off + k, tg * 512:(tg + 1) * 512], start=(k == 0), stop=(k == nk - 1)),
                                      rd=[sl.R(), attnT.R()], wr=[b.R()])
                            g_ap = gt[:, fc2, tg * 512:(tg + 1) * 512]
                            ma = macc[:, fc2 * 2 + tg, :]
                            mres = macc.R(fc2 * 2 + tg)
                            if gi == 0:
                                kb.op("dve", lambda h: h.tensor_tensor(out=ma, in0=b.t[:, :], in1=g_ap, op=ALU.mult),
                                      rd=[b.R(), gt.R()], wr=[mres])
                            else:
                                tm = tmpr.next()
                                kb.op("dve", lambda h: h.tensor_tensor(out=tm[:], in0=b.t[:, :], in1=g_ap, op=ALU.mult),
                                      rd=[b.R(), gt.R()], wr=[tm.R()])
                                if gi == 1:
                                    kb.op("pool", lambda h: h.tensor_tensor(out=ma, in0=ma, in1=tm[:], op=ALU.add),
                                          rd=[tm.R(), mres], wr=[mres])
                                else:
                                    kb.op("pool", lambda h: h.tensor_tensor(out=mergedT[:, fp * 2 + fc2, tg * 512:(tg + 1) * 512],
                                                                            in0=ma, in1=tm[:], op=ALU.add),
                                          rd=[tm.R(), mres], wr=[mergedT.R()])
            w_o = din["w_o"].t[0]
            for cs in range(8):
                sl, sv = cx.load_slab(w_o[:, cs * 256:(cs + 1) * 256], 16, 256)
                for i in range(NT):
                    b = cx.bank()
                    for k in range(16):
                        kb.op("pe", lambda h: h.matmul(b.t[:, 0:256], lhsT=mergedT[:, k, i * 128:(i + 1) * 128], rhs=sv[:, k, :],
                                                       start=(k == 0), stop=(k == 15)), rd=[sl.R(), mergedT.R()], wr=[b.R()])
                    xb = xblk.next()
                    kb.op("sp", lambda h: h.dma_start(out=xb[:], in_=xs_d.t[i * 128:(i + 1) * 128, cs * 256:(cs + 1) * 256]),
                          rd=[xs_d.R()], wr=[xb.R()], dma=True)
                    kb.op("dve", lambda h: h.scalar_tensor_tensor(out=xb[:], in0=xb[:], scalar=ALPHA, in1=b.t[:, 0:256],
                                                                  op0=ALU.mult, op1=ALU.add), rd=[xb.R(), b.R()], wr=[xb.R()])
                    kb.op("sp", lambda h: h.dma_start(out=d_x1pre.t[i * 128:(i + 1) * 128, cs * 256:(cs + 1) * 256], in_=xb[:]),
                          rd=[xb.R()], wr=[d_x1pre.R()], dma=True)
            kb.barrier()
        x1T = kb.sb(st, "x1T", [128, 16, 1024], BF16)
        acc = kb.sb(st, "acc", [128, 8, 2048], F32)
        comb = kb.sb(st, "comb", [128, 8, 32], F32)
        hidr = Ring([kb.sb(st, "hid", [128, 4, 1024], BF16) for _ in range(2)])
        silr = Ring([kb.sb(st, "sil", [128, 512], BF16) for _ in range(3)])
        lg = kb.sb(st, "lg", [128, 36], F32)
        rt = kb.sb(st, "rt", [128, 64], F32)
        top8 = kb.sb(st, "top8c", [128, 8], F32)
        junk = Tile(hidr.tiles[0].t[:, 0:2, :])
        junk.res = hidr.tiles[0].res
        gb = load_gb(cx, din["ln1_g"], din["ln1_b"], l)
        for i in range(NT):
            xa = acc[:, i, :]
            kb.op("sp", lambda h: h.dma_start(out=xa, in_=d_x1pre.t[i * 128:(i + 1) * 128, :]), rd=[d_x1pre.R()], wr=[acc.R(i)], dma=True)
            layer_norm_tile(cx, xa, acc.R(i), gb, gb.R(), smr.next(), junk)
            for g0 in range(0, 16, 4):
                b = cx.bank()
                for q in range(4):
                    kb.op("pe", lambda h: h.transpose(out=b.t[:, q * 128:(q + 1) * 128], in_=acc[:, i, (g0 + q) * 128:(g0 + q + 1) * 128],
                                                      identity=C["ident"][:]), rd=[acc.R(i), C["ident"].R()], wr=[b.R()])
                cx.copy(cx.evac_eng(), x1T[:, g0:g0 + 4, i * 128:(i + 1) * 128], b.t[:, :].rearrange("p (n t) -> p n t", t=128),
                        [b.R()], [x1T.R()])
        slr, svr = cx.load_slab(din["w_r"].t[0], 16, 36)
        for i in range(NT):
            b = cx.bank()
            for k in range(16):
                kb.op("pe", lambda h: h.matmul(b.t[:, 0:36], lhsT=x1T[:, k, i * 128:(i + 1) * 128], rhs=svr[:, k, :],
                                               start=(k == 0), stop=(k == 15)), rd=[slr.R(), x1T.R()], wr=[b.R()])
            R_ = [lg.R(), rt.R(), top8.R()]
            def dv(fn, rd=R_, wr=R_):
                kb.op("dve", fn, rd=rd, wr=wr)
            dv(lambda h: h.tensor_tensor(out=lg[:], in0=b.t[:, 0:36], in1=C["b_r"][:, l, :], op=ALU.add), rd=[b.R(), C["b_r"].R()] + R_)
            dv(lambda h: h.tensor_reduce(out=rt[:, 0:1], in_=lg[:, 0:4], axis=AX.X, op=ALU.max))
            dv(lambda h: h.tensor_scalar(out=rt[:, 4:8], in0=lg[:, 0:4], scalar1=rt[:, 0:1], scalar2=None, op0=ALU.is_equal))
            dv(lambda h: h.tensor_scalar(out=rt[:, 1:2], in0=rt[:, 0:1], scalar1=-1.0, scalar2=None, op0=ALU.mult))
            kb.op("act", lambda h: h.activation(out=rt[:, 8:12], in_=lg[:, 0:4], func=AF.Exp, bias=rt[:, 1:2], accum_out=rt[:, 2:3]),
                  rd=R_, wr=R_)
            dv(lambda h: h.reciprocal(out=rt[:, 3:4], in_=rt[:, 2:3]))
            dv(lambda h: h.tensor_scalar(out=rt[:, 16:24], in0=lg[:, 4:12], scalar1=rt[:, 4:5], scalar2=None, op0=ALU.mult))
            for g in range(1, 4):
                dv(lambda h: h.scalar_tensor_tensor(out=rt[:, 16:24], in0=lg[:, 4 + 8 * g:12 + 8 * g], scalar=rt[:, 4 + g:5 + g],
                                                    in1=rt[:, 16:24], op0=ALU.mult, op1=ALU.add))
            dv(lambda h: h.max(out=top8[:], in_=rt[:, 16:24]))
            dv(lambda h: h.tensor_tensor(out=rt[:, 12:13], in0=top8[:, 1:2], in1=top8[:, 0:1], op=ALU.subtract))
            kb.op("act", lambda h: h.activation(out=rt[:, 13:14], in_=rt[:, 12:13], func=AF.Exp), rd=R_, wr=R_)
            dv(lambda h: h.tensor_scalar(out=rt[:, 14:15], in0=rt[:, 13:14], scalar1=1.0, scalar2=None, op0=ALU.add))
            dv(lambda h: h.reciprocal(out=rt[:, 14:15], in_=rt[:, 14:15]))
            dv(lambda h: h.tensor_tensor(out=rt[:, 15:16], in0=rt[:, 13:14], in1=rt[:, 14:15], op=ALU.mult))
            dv(lambda h: h.tensor_scalar(out=rt[:, 24:32], in0=rt[:, 16:24], scalar1=top8[:, 0:1], scalar2=rt[:, 14:15],
                                         op0=ALU.is_equal, op1=ALU.mult))
            dv(lambda h: h.tensor_scalar(out=rt[:, 32:40], in0=rt[:, 16:24], scalar1=top8[:, 1:2], scalar2=rt[:, 15:16],
                                         op0=ALU.is_equal, op1=ALU.mult))
            dv(lambda h: h.tensor_tensor(out=rt[:, 24:32], in0=rt[:, 24:32], in1=rt[:, 32:40], op=ALU.add))
            dv(lambda h: h.tensor_scalar(out=rt[:, 24:32], in0=rt[:, 24:32], scalar1=rt[:, 3:4], scalar2=None, op0=ALU.mult))
            for g in range(4):
                kb.op("dve", lambda h: h.tensor_scalar(out=comb[:, i, 8 * g:8 * g + 8], in0=rt[:, 24:32], scalar1=rt[:, 4 + g:5 + g],
                                                       scalar2=None, op0=ALU.mult), rd=R_, wr=[comb.R()])
            kb.op("pool", lambda h: h.tensor_scalar(out=acc[:, i, :], in0=acc[:, i, :], scalar1=ALPHA, scalar2=None, op0=ALU.mult),
                  rd=[acc.R(i)], wr=[acc.R(i)])
        for e in range(32):
            hid = hidr.next()
            w1e = din["w1"].t[0, e]; w3e = din["w3"].t[0, e]; w2e = din["w2"].t[0, e]
            for fh in range(2):
                s1_, v1 = cx.load_slab(w1e[:, fh * 256:(fh + 1) * 256], 16, 256)
                s3_, v3 = cx.load_slab(w3e[:, fh * 256:(fh + 1) * 256], 16, 256)
                for fc2 in range(2):
                    for tg in range(2):
                        b1 = cx.bank(); b3 = cx.bank()
                        for (bb, ss_, vv_) in ((b1, s1_, v1), (b3, s3_, v3)):
                            for k in range(16):
                                kb.op("pe", lambda h: h.matmul(bb.t[:, :], lhsT=vv_[:, k, fc2 * 128:(fc2 + 1) * 128],
                                                               rhs=x1T[:, k, tg * 512:(tg + 1) * 512], start=(k == 0), stop=(k == 15)),
                                      rd=[ss_.R(), x1T.R()], wr=[bb.R()])
                        sil = silr.next()
                        kb.op("act", lambda h: h.activation(out=sil[:], in_=b1.t[:, :], func=AF.Silu), rd=[b1.R()], wr=[sil.R()])
                        kb.op("dve", lambda h: h.tensor_tensor(out=hid[:, fh * 2 + fc2, tg * 512:(tg + 1) * 512], in0=b3.t[:, :], in1=sil[:],
                                                               op=ALU.mult), rd=[b3.R(), sil.R()], wr=[hid.R()])
            for half in range(2):
                s2_, v2 = cx.load_slab(w2e[:, half * 1024:(half + 1) * 1024], 4, 1024)
                for i in range(NT):
                    for cc in range(2):
                        b = cx.bank()
                        for k in range(4):
                            kb.op("pe", lambda h: h.matmul(b.t[:, :], lhsT=hid[:, k, i * 128:(i + 1) * 128], rhs=v2[:, k, cc * 512:(cc + 1) * 512],
                                                           start=(k == 0), stop=(k == 3)), rd=[s2_.R(), hid.R()], wr=[b.R()])
                        c0 = half * 1024 + cc * 512
                        kb.op("dve", lambda h: h.scalar_tensor_tensor(out=acc[:, i, c0:c0 + 512], in0=b.t[:, :], scalar=comb[:, i, e:e + 1],
                                                                      in1=acc[:, i, c0:c0 + 512], op0=ALU.mult, op1=ALU.add),
                              rd=[b.R(), comb.R(), acc.R(i)], wr=[acc.R(i)])
        gb = load_gb(cx, din["ln2_g"], din["ln2_b"], l)
        for i in range(NT):
            layer_norm_tile(cx, acc[:, i, :], acc.R(i), gb, gb.R(), smr.next(), junk)
            kb.op("sp", lambda h: h.dma_start(out=d_out.t[i * 128:(i + 1) * 128, :], in_=acc[:, i, :]), rd=[acc.R(i)], wr=[d_out.R()], dma=True)
    kb.barrier()


CONSTS_A = {"ident": ([128, 128], F32), "epsc": ([128, 1], F32),
            "g_cq": ([128, DEPTH, 512], F32), "g_ckv": ([128, DEPTH, 256], F32),
            "g_kidx": ([128, DEPTH, 64], F32), "b_kidx": ([128, DEPTH, 64], F32)}
CONSTS_BC = {"ident": ([128, 128], F32), "epsc": ([128, 1], F32),
             "pos_col": ([128, 8], F32), "cpos": ([128, 8], F32), "pos_row": ([128, 1024], F32),
             "iota256": ([128, 256], F32), "iota_p": ([128, 1], F32), "slopes": ([128, 12], F32),
             "ucol": ([128, 32], F32), "ustart": ([128, 4], F32), "abias": ([128, 12], F32), "iota256p1": ([128, 256], F32),
             "b_gate": ([128, DEPTH, 48], F32), "b_r": ([128, DEPTH, 36], F32)}

QSHAPES = {"qiT": [128, 9, 1024], "qaT": [128, 6, 1024], "qbT": [128, 6, 1024], "qcT": [128, 4, 1024]}
KSHAPES = {"kiT": [64, 1024], "kaT": [128, 6, 1024], "va": [128, 8, 774], "kbT": [128, 6, 1024],
           "vb": [128, 8, 774], "kmT": [128, 4, 6]}
WSH = {"w_in": [1, 2048, 3664], "w_uq": [1, 512, 768], "w_uqi": [1, 512, 1024],
       "w_ukv": [1, 256, 1536], "w_up_a": [1, 768, 2048], "w_up_b": [1, 768, 2048],
       "w_up_c": [1, 512, 2048], "w_gate": [1, 2048, 6144], "w_o": [1, 2048, 2048],
       "w1": [1, 32, 2048, 512], "w3": [1, 32, 2048, 512], "w2": [1, 32, 512, 2048],
       "w_mem_kv": [2048, 1024], "mem": [256, 2048], "w_r": [1, 2048, 36],
       "ln1_g": [128, DEPTH, 2048], "ln1_b": [128, DEPTH, 2048], "ln2_g": [128, DEPTH, 2048], "ln2_b": [128, DEPTH, 2048]}
A_W = ("w_in", "w_uq", "w_uqi", "w_ukv")
BC_W = ("w_up_a", "w_up_b", "w_up_c", "w_gate", "w_o", "w1", "w3", "w2", "w_mem_kv", "mem", "w_r",
        "ln1_g", "ln1_b", "ln2_g", "ln2_b")


def build_A(l):
    kb = KB()
    with ExitStack() as stack:
        cx = Ctx(kb, stack, CONSTS_A)
        din = {n: kb.dram(n, WSH[n], F32, "ExternalInput") for n in A_W}
        xs = kb.dram("xs", [TL, D], F32, "ExternalInput")
        dq = {n: kb.dram(n, s, BF16, "ExternalOutput") for n, s in QSHAPES.items()}
        dq["wsg"] = kb.dram("wsg", [128, 8, 16], F32, "ExternalOutput")
        dk = {n: kb.dram(n, s, BF16, "ExternalOutput") for n, s in KSHAPES.items()}
        phase_A(cx, l, xs, din, dq, dk)
        kb.final_wait()
    return kb


def build_BC(l, with_next_A, stop_after=None, debug=False):
    kb = KB()
    with ExitStack() as stack:
        consts = dict(CONSTS_BC)
        if with_next_A:
            consts.update(CONSTS_A)
        cx = Ctx(kb, stack, consts)
        names = BC_W + (A_W if with_next_A else ())
        din = {n: kb.dram(n, WSH[n], F32, "ExternalInput") for n in names}
        xs = kb.dram("xs", [TL, D], F32, "ExternalInput")
        dq = {n: kb.dram(n + "_in", s, BF16, "ExternalInput") for n, s in QSHAPES.items()}
        dq["wsg"] = kb.dram("wsg_in", [128, 8, 16], F32, "ExternalInput")
        dka = {n: kb.dram(n + "_all", [8] + s, BF16, "ExternalInput") for n, s in KSHAPES.items()}
        d_attn = kb.dram("attn_scr", [128, 16, 1024], BF16, "ExternalOutput" if (stop_after == "B" or debug) else "Internal")
        if stop_after != "B":
            d_x1pre = kb.dram("x1pre_scr", [TL, D], F32, "ExternalOutput" if debug else "Internal")
        d_out = kb.dram("xout", [TL, D], F32, "ExternalOutput")
        mkT, mv = phase_M(cx, stack, din)
        phase_B(cx, l, dq, dka, d_attn, mkT, mv)
        if stop_after != "B":
            phase_C(cx, l, xs, d_attn, din, d_x1pre, d_out)
            if with_next_A:
                dq2 = {n: kb.dram(n, s, BF16, "ExternalOutput") for n, s in QSHAPES.items()}
                dq2["wsg"] = kb.dram("wsg", [128, 8, 16], F32, "ExternalOutput")
                dk2 = {n: kb.dram(n, s, BF16, "ExternalOutput") for n, s in KSHAPES.items()}
                phase_A(cx, l + 1, d_out, din, dq2, dk2)
        kb.final_wait()
    return kb


def core_rows(c):
    return np.concatenate([np.arange(unit_of(c, j) * 256, unit_of(c, j) * 256 + 256) for j in range(4)])


def bcast(v):
    v = np.asarray(v, dtype=np.float32)
    return np.ascontiguousarray(np.broadcast_to(v[None], (128,) + v.shape))


def host_consts(inp, c):
    rows = core_rows(c).astype(np.float32)
    slopes = 2.0 ** (-8.0 * np.arange(1, 13, dtype=np.float32) / 12)
    sl12 = np.concatenate([slopes[0::2], slopes[1::2]]).astype(np.float32)
    ucol = np.array([256.0 * unit_of(q % 8, q // 8) for q in range(32)], np.float32)
    ustart = np.array([256.0 * unit_of(c, j) for j in range(4)], np.float32)
    b_r = np.concatenate([inp["b_grp"], inp["b_rt"]], axis=-1)
    cst = {
        "ident": np.eye(128, dtype=np.float32), "epsc": np.full((128, 1), EPS, np.float32),
        "g_cq": bcast(inp["g_cq"]), "g_ckv": bcast(inp["g_ckv"]), "g_kidx": bcast(inp["g_kidx"]), "b_kidx": bcast(inp["b_kidx"]),
        "pos_col": np.ascontiguousarray(rows.reshape(8, 128).T), "cpos": bcast(rows.reshape(8, 128)[:, 0] + 64.0),
        "pos_row": bcast(rows), "iota256": bcast(np.arange(256, dtype=np.float32)),
        "iota_p": np.arange(128, dtype=np.float32).reshape(128, 1), "slopes": bcast(sl12),
        "ucol": bcast(ucol), "ustart": bcast(ustart),
        "abias": np.ascontiguousarray((np.arange(128, dtype=np.float32)[:, None] - 127.0) * sl12[None, :]),
        "iota256p1": bcast(np.arange(1, 257, dtype=np.float32)),
        "b_gate": np.ascontiguousarray(np.asarray(inp["b_gate"], np.float32).reshape(DEPTH, 48, 128).transpose(2, 0, 1)),
        "b_r": bcast(b_r),
        "ln1_g": bcast(inp["ln1_g"]), "ln1_b": bcast(inp["ln1_b"]), "ln2_g": bcast(inp["ln2_g"]), "ln2_b": bcast(inp["ln2_b"]),
    }
    return cst


def layer_weights(inp, l):
    w = {n: np.asarray(inp[n][l:l + 1], np.float32) for n in ("w_in", "w_uq", "w_uqi", "w_ukv", "w_up_a", "w_up_b", "w_up_c",
                                                              "w_gate", "w_o", "w1", "w3", "w2")}
    w["w_r"] = np.concatenate([inp["w_grp"][l:l + 1], inp["w_rt"][l:l + 1]], axis=-1).astype(np.float32)
    w["w_mem_kv"] = np.asarray(inp["w_mem_kv"], np.float32)
    w["mem"] = np.asarray(inp["mem"][0], np.float32)
    return w


_CACHE = {}


def get_prog(key, fn):
    if key not in _CACHE:
        _CACHE[key] = fn()
    return _CACHE[key]


def run_A(inp, l, xs_list):
    kb = get_prog(("A", l), lambda: build_A(l))
    w = layer_weights(inp, l)
    maps = []
    for c in range(NCORE):
        cst = host_consts(inp, c)
        m = {n: cst[n] for n in CONSTS_A}
        m.update({n: w[n] for n in A_W})
        m["xs"] = xs_list[c]
        maps.append(m)
    res = run_bass_kernel_spmd(kb.nc, maps, core_ids=list(range(NCORE)))
    return res.results


def run_BC(inp, l, xs_list, qk, with_next_A, stop_after=None):
    kb = get_prog(("BC", l, with_next_A, stop_after), lambda: build_BC(l, with_next_A, stop_after))
    w = layer_weights(inp, l)
    if with_next_A:
        w.update({n: np.asarray(inp[n][l + 1:l + 2], np.float32) for n in A_W})
    kall = {n + "_all": np.ascontiguousarray(np.stack([np.asarray(qk[c][n]) for c in range(NCORE)])) for n in KSHAPES}
    maps = []
    for c in range(NCORE):
        cst = host_consts(inp, c)
        m = {n: cst[n] for n in CONSTS_BC}
        if with_next_A:
            m.update({n: cst[n] for n in CONSTS_A})
        for n in BC_W + (A_W if with_next_A else ()):
            m[n] = cst[n] if n.startswith("ln") else w[n]
        m["xs"] = xs_list[c]
        for n in QSHAPES:
            m[n + "_in"] = np.asarray(qk[c][n])
        m["wsg_in"] = np.asarray(qk[c]["wsg"])
        m.update(kall)
        maps.append(m)
    res = run_bass_kernel_spmd(kb.nc, maps, core_ids=list(range(NCORE)))
    return res.results


def kernel(**inp):
    x = np.asarray(inp["x"], np.float32)[0]
    xs_list = [np.ascontiguousarray(x[core_rows(c)]) for c in range(NCORE)]
    r = run_A(inp, 0, xs_list)
    r = run_BC(inp, 0, xs_list, r, True)
    xs_list = [np.asarray(r[c]["xout"]) for c in range(NCORE)]
    r = run_BC(inp, 1, xs_list, r, False)
    out = np.zeros((1, T, D), np.float32)
    for c in range(NCORE):
        out[0, core_rows(c)] = np.asarray(r[c]["xout"])
    return out
```

```python
import numpy as np
import ml_dtypes
from contextlib import ExitStack
import concourse.bass as bass
import concourse.mybir as mybir
from concourse.bass_utils import run_bass_kernel_spmd

F32 = mybir.dt.float32
BF16 = mybir.dt.bfloat16
AF = mybir.ActivationFunctionType
ALU = mybir.AluOpType
AX = mybir.AxisListType

NCORE = 8
D = 2048
T = 8192
TL = 1024
NT = 8
DEPTH = 2
NEG = -1.0e30
ALPHA = (2 * DEPTH) ** 0.25
EPS = 1e-5
N_BISECT = 27
SCALE = 128 ** -0.5


def unit_of(r, j):
    return [r, 15 - r, 16 + r, 31 - r][j]


class Sem:
    def __init__(self, h):
        self.h = h
        self.n = 0


class Res:
    __slots__ = ("w", "r")

    def __init__(self):
        self.w = None
        self.r = {}


class Tile:
    def __init__(self, t):
        self.t = t
        self.res = Res()
        self.sub = {}

    def __getitem__(self, k):
        return self.t[k]

    def R(self, key=None):
        if key is None:
            return self.res
        if key not in self.sub:
            self.sub[key] = Res()
        return self.sub[key]


class Eng:
    def __init__(self, kb, name, h):
        self.kb = kb
        self.name = name
        self.h = h
        self.c = None
        self.d = None
        self.seen = {}

    def csem(self):
        if self.c is None or self.c.n > 30000:
            self.c = Sem(self.kb.new_sem())
            self.kb.allsems.append(self.c)
        return self.c

    def dsem(self):
        if self.d is None or self.d.n > 30000:
            self.d = Sem(self.kb.new_sem())
            self.kb.allsems.append(self.d)
        return self.d


class KB:
    def __init__(self):
        self.nc = bass.Bass("TRN2", target_bir_lowering=False)
        self.es = ExitStack()
        self.allsems = []
        nc = self.nc
        self.E = {
            "pe": Eng(self, "pe", nc.tensor),
            "act": Eng(self, "act", nc.scalar),
            "dve": Eng(self, "dve", nc.vector),
            "pool": Eng(self, "pool", nc.gpsimd),
            "sp": Eng(self, "sp", nc.sync),
        }
        self.nid = 0
        self.ninst = 0

    def new_sem(self):
        self.nid += 1
        return self.es.enter_context(self.nc.semaphore("s%d" % self.nid))

    def dram(self, name, shape, dt, kind):
        t = self.nc.dram_tensor(name, list(shape), dt, kind=kind)
        return Tile(t)

    def sb(self, stack, name, shape, dt):
        self.nid += 1
        t = stack.enter_context(self.nc.sbuf_tensor("%s_%d" % (name, self.nid), list(shape), dt))
        return Tile(t)

    def ps(self, stack, name, shape, dt):
        self.nid += 1
        t = stack.enter_context(self.nc.psum_tensor("%s_%d" % (name, self.nid), list(shape), dt))
        return Tile(t)

    def op(self, e, fn, rd=(), wr=(), dma=False):
        E = self.E[e]
        deps = {}

        def add(tok):
            s, v = tok
            if deps.get(s, 0) < v:
                deps[s] = v

        for r in rd:
            if r.w is not None:
                add(r.w)
        for r in wr:
            if r.w is not None:
                add(r.w)
            for s, v in r.r.items():
                add((s, v))
        for s, v in deps.items():
            if e == "pe" and (not dma) and s is E.c:
                continue
            if E.seen.get(s, 0) >= v:
                continue
            E.h.wait_ge(s.h, v)
            E.seen[s] = v
        ins = fn(E.h)
        self.ninst += 1
        if dma:
            S = E.dsem()
            S.n += 16
            ins.then_inc(S.h, 16)
        else:
            S = E.csem()
            S.n += 1
            ins.then_inc(S.h, 1)
        tok = (S, S.n)
        for r in rd:
            if r.r.get(S, 0) < S.n:
                r.r[S] = S.n
        for r in wr:
            r.w = tok
            r.r = {}
        return ins

    def barrier(self):
        for E in self.E.values():
            for S in self.allsems:
                if S.n > 0 and E.seen.get(S, 0) < S.n:
                    E.h.wait_ge(S.h, S.n)
                    E.seen[S] = S.n

    def final_wait(self):
        E = self.E["sp"]
        for S in self.allsems:
            if S.n > 0 and E.seen.get(S, 0) < S.n:
                E.h.wait_ge(S.h, S.n)
                E.seen[S] = S.n


class Ring:
    def __init__(self, tiles):
        self.tiles = tiles
        self.i = 0

    def next(self):
        t = self.tiles[self.i % len(self.tiles)]
        self.i += 1
        return t


class Ctx:
    def __init__(self, kb, stack, consts):
        self.kb = kb
        nc = kb.nc
        self.banks = [kb.ps(stack, "bank", [128, 512], F32) for _ in range(8)]
        self.bank_i = 0
        self.slabs = Ring([kb.sb(stack, "slab", [128, 4096], BF16) for _ in range(4)])
        self.stage = Ring([kb.sb(stack, "stage", [128, 4096], F32) for _ in range(2)])
        self.cst = {}
        for name, (shape, dt) in consts.items():
            d = kb.dram(name, shape, dt, "ExternalInput")
            t = kb.sb(stack, name, shape, dt)
            kb.op("sp", lambda h, t=t, d=d: h.dma_start(out=t[:], in_=d[:]), wr=[t.R()], dma=True)
            self.cst[name] = t
        self.ident_bf = kb.sb(stack, "identbf", [128, 128], BF16)
        kb.op("dve", lambda h: h.tensor_copy(out=self.ident_bf[:], in_=self.cst["ident"][:]),
              rd=[self.cst["ident"].R()], wr=[self.ident_bf.R()])
        self.evac_i = 0

    def bank(self):
        b = self.banks[self.bank_i % 8]
        self.bank_i += 1
        return b

    def evac_eng(self):
        self.evac_i += 1
        return "act" if self.evac_i % 2 else "dve"

    def copy(self, eng, out_ap, in_ap, rd, wr):
        kb = self.kb
        if eng == "act":
            kb.op("act", lambda h: h.activation(out=out_ap, in_=in_ap, func=AF.Copy), rd=rd, wr=wr)
        else:
            kb.op(eng, lambda h: h.tensor_copy(out=out_ap, in_=in_ap), rd=rd, wr=wr)

    def load_slab(self, src_ap, nk, C):
        kb = self.kb
        assert nk * C <= 4096
        st = self.stage.next()
        sl = self.slabs.next()
        sv = st.t[:, 0:nk * C].rearrange("p (k c) -> p k c", c=C)
        kb.op("sp", lambda h: h.dma_start(out=sv, in_=src_ap.rearrange("(k p) c -> p k c", p=128)),
              wr=[st.R()], dma=True)
        kb.op("pool", lambda h: h.tensor_copy(out=sl.t[:, 0:nk * C], in_=st.t[:, 0:nk * C]),
              rd=[st.R()], wr=[sl.R()])
        return sl, sl.t[:, 0:nk * C].rearrange("p (k c) -> p k c", c=C)

    def transpose_to(self, src_tile, src_aps, dst_fn, dst_res, dt, rows=128):
        kb = self.kb
        per = 4 if dt == F32 else 8
        ident = self.cst["ident"] if dt == F32 else self.ident_bf
        for g0 in range(0, len(src_aps), per):
            n = min(per, len(src_aps) - g0)
            b = self.bank()
            bv = b.t[:] if dt == F32 else b.t[:].bitcast(BF16)
            for q in range(n):
                kb.op("pe", lambda h, q=q: h.transpose(out=bv[0:rows, q * 128:(q + 1) * 128],
                                                        in_=src_aps[g0 + q], identity=ident[:]),
                      rd=[src_tile.R(), ident.R()], wr=[b.R()])
            self.copy(self.evac_eng(), dst_fn(g0, n),
                      bv[0:rows, 0:n * 128].rearrange("p (n t) -> p n t", t=128), [b.R()], [dst_res])


def W2(t, l):
    return t.t[l]


def phase_M(cx, stack, din):
    kb = cx.kb
    mkT = kb.sb(stack, "mkT", [128, 4, 256], BF16)
    mv = kb.sb(stack, "mv", [128, 2, 4, 129], BF16)
    with ExitStack() as st:
        memT = kb.sb(st, "memT", [128, 16, 256], BF16)
        mem_in = kb.sb(st, "memin", [128, 2048], F32)
        for mt in range(2):
            kb.op("sp", lambda h: h.dma_start(out=mem_in[:], in_=din["mem"].t[mt * 128:(mt + 1) * 128, :]),
                  wr=[mem_in.R()], dma=True)
            cx.transpose_to(mem_in, [mem_in[:, c * 128:(c + 1) * 128] for c in range(16)],
                            lambda g0, n: memT[:, g0:g0 + n, mt * 128:(mt + 1) * 128], memT.R(), F32)
        kb.op("pool", lambda h: h.memset(mv[:], 1.0), wr=[mv.R()])
        for cs in range(4):
            sl, sv = cx.load_slab(din["w_mem_kv"].t[:, cs * 256:(cs + 1) * 256], 16, 256)
            if cs < 2:
                for hh in range(2):
                    b = cx.bank()
                    for k in range(16):
                        kb.op("pe", lambda h: h.matmul(b.t[:, 0:256], lhsT=sv[:, k, hh * 128:(hh + 1) * 128],
                                                       rhs=memT[:, k, :], start=(k == 0), stop=(k == 15)),
                              rd=[sl.R(), memT.R()], wr=[b.R()])
                    cx.copy(cx.evac_eng(), mkT[:, cs * 2 + hh, :], b.t[:, 0:256], [b.R()], [mkT.R()])
            else:
                for mt in range(2):
                    b = cx.bank()
                    for k in range(16):
                        kb.op("pe", lambda h: h.matmul(b.t[:, 0:256], lhsT=memT[:, k, mt * 128:(mt + 1) * 128],
                                                       rhs=sv[:, k, :], start=(k == 0), stop=(k == 15)),
                              rd=[sl.R(), memT.R()], wr=[b.R()])
                    h0 = (cs - 2) * 2
                    cx.copy(cx.evac_eng(), mv[:, mt, h0:h0 + 2, 0:128],
                            b.t[:, 0:256].rearrange("p (a d) -> p a d", d=128), [b.R()], [mv.R()])
    return mkT, mv


W_IN_SLABS = [(0, 256), (256, 256), (512, 256), (768, 80)] + \
             [(848 + 256 * i, 256) for i in range(3)] + [(1616 + 256 * i, 256) for i in range(3)] + \
             [(2384 + 256 * i, 256) for i in range(3)] + [(3152 + 256 * i, 256) for i in range(2)]


def phase_A(cx, l, xs_d, din, dq, dk):
    kb = cx.kb
    with ExitStack() as st:
        Hs = kb.sb(st, "Hs", [128, 8, 848], F32)
        fm = Ring([kb.sb(st, "fm", [128, 6, 1024], BF16) for _ in range(2)])
        vv = kb.sb(st, "vv", [128, 8, 6, 129], BF16)
        kb.op("pool", lambda h: h.memset(vv[:], 1.0), wr=[vv.R()])
        kms = kb.sb(st, "kms", [128, 4, 6], F32)
        kmb = kb.sb(st, "kmb", [128, 4, 6], BF16)
        st1 = ExitStack()
        xT = kb.sb(st1, "xT", [128, 16, 1024], BF16)
        qbT = fm.next(); kbT = fm.next(); qcT = None
        vb = vv; va = vv
        for i in range(NT):
            xi = cx.stage.next()
            kb.op("sp", lambda h: h.dma_start(out=xi[:, 0:2048], in_=xs_d.t[i * 128:(i + 1) * 128, :]),
                  rd=[xs_d.R()], wr=[xi.R()], dma=True)
            cx.transpose_to(xi, [xi[:, c * 128:(c + 1) * 128] for c in range(16)],
                            lambda g0, n: xT[:, g0:g0 + n, i * 128:(i + 1) * 128], xT.R(), F32)
        w_in = din["w_in"].t[0]
        for si, (c0, C) in enumerate(W_IN_SLABS):
            sl, sv = cx.load_slab(w_in[:, c0:c0 + C], 16, C)
            if si < 4:
                for i in range(NT):
                    b = cx.bank()
                    for k in range(16):
                        kb.op("pe", lambda h: h.matmul(b.t[:, 0:C], lhsT=xT[:, k, i * 128:(i + 1) * 128],
                                                       rhs=sv[:, k, :], start=(k == 0), stop=(k == 15)),
                              rd=[sl.R(), xT.R()], wr=[b.R()])
                    cx.copy(cx.evac_eng(), Hs[:, i, c0:c0 + C], b.t[:, 0:C], [b.R()], [Hs.R(i)])
            elif 10 <= si < 13:
                h0 = (si - 10) * 2
                for i in range(NT):
                    b = cx.bank()
                    for k in range(16):
                        kb.op("pe", lambda h: h.matmul(b.t[:, 0:256], lhsT=xT[:, k, i * 128:(i + 1) * 128],
                                                       rhs=sv[:, k, :], start=(k == 0), stop=(k == 15)),
                              rd=[sl.R(), xT.R()], wr=[b.R()])
                    cx.copy(cx.evac_eng(), vb[:, i, h0:h0 + 2, 0:128],
                            b.t[:, 0:256].rearrange("p (a d) -> p a d", d=128), [b.R()], [vb.R()])
            else:
                if si < 7:
                    dst, h0 = qbT, (si - 4) * 2
                elif si < 10:
                    dst, h0 = kbT, (si - 7) * 2
                else:
                    if si == 13:
                        qcT = fm.next()
                    dst, h0 = qcT, (si - 13) * 2
                for hh in range(2):
                    for tg in range(2):
                        b = cx.bank()
                        for k in range(16):
                            kb.op("pe", lambda h: h.matmul(b.t[:, :], lhsT=sv[:, k, hh * 128:(hh + 1) * 128],
                                                           rhs=xT[:, k, tg * 512:(tg + 1) * 512],
                                                           start=(k == 0), stop=(k == 15)),
                                  rd=[sl.R(), xT.R()], wr=[b.R()])
                        cx.copy(cx.evac_eng(), dst[:, h0 + hh, tg * 512:(tg + 1) * 512], b.t[:, :],
                                [b.R()], [dst.R()])
                if si == 6:
                    kb.op("sp", lambda h: h.dma_start(out=dq["qbT"].t[:], in_=qbT[:]), rd=[qbT.R()], wr=[dq["qbT"].R()], dma=True)
                if si == 14:
                    kb.op("sp", lambda h: h.dma_start(out=dq["qcT"].t[:], in_=qcT[:, 0:4, :]), rd=[qcT.R()], wr=[dq["qcT"].R()], dma=True)
            if si == 12:
                kb.op("sp", lambda h: h.dma_start(out=dk["vb"].t[:], in_=vb[:].rearrange("p i h d -> p i (h d)")),
                      rd=[vb.R()], wr=[dk["vb"].R()], dma=True)
        kb.op("sp", lambda h: h.dma_start(out=dk["kbT"].t[:], in_=kbT[:]), rd=[kbT.R()], wr=[dk["kbT"].R()], dma=True)
        for j in range(4):
            kb.op("dve", lambda h: h.tensor_reduce(out=kms[:, j, :], in_=kbT[:, :, j * 256:(j + 1) * 256],
                                                   axis=AX.X, op=ALU.add), rd=[kbT.R()], wr=[kms.R()])
        kb.op("dve", lambda h: h.tensor_scalar(out=kmb[:], in0=kms[:], scalar1=1.0 / 256, scalar2=None, op0=ALU.mult),
              rd=[kms.R()], wr=[kmb.R()])
        kb.op("sp", lambda h: h.dma_start(out=dk["kmT"].t[:], in_=kmb[:]), rd=[kmb.R()], wr=[dk["kmT"].R()], dma=True)

        kb.barrier()
        st1.close()
        cqnT = kb.sb(st, "cqnT", [128, 4, 1024], BF16)
        ckvnT = kb.sb(st, "ckvnT", [128, 2, 1024], BF16)
        kiT = kb.sb(st, "kiT", [64, 1024], BF16)
        wS = kb.sb(st, "wS", [128, 8, 16], F32)
        wN = kb.sb(st, "wN", [128, 8, 16], F32)
        wG = kb.sb(st, "wG", [128, 8, 16], F32)
        junk = kb.sb(st, "junk", [128, 512], F32)
        cqn = Ring([kb.sb(st, "cqn", [128, 768], BF16) for _ in range(2)])
        sm = Ring([kb.sb(st, "sm", [128, 16], F32) for _ in range(4)])
        kin = Ring([kb.sb(st, "kin", [128, 64], F32) for _ in range(2)])
        kinb = Ring([kb.sb(st, "kinb", [128, 64], BF16) for _ in range(2)])
        gq = cx.cst["g_cq"]; gkv = cx.cst["g_ckv"]; gki = cx.cst["g_kidx"]; bki = cx.cst["b_kidx"]
        for i in range(NT):
            s = sm.next()
            cq = cqn.next()
            for (c0, n, col, g) in ((0, 512, 0, gq), (512, 256, 1, gkv)):
                kb.op("act", lambda h: h.activation(out=junk[:, 0:n], in_=Hs[:, i, c0:c0 + n], func=AF.Square,
                                                    accum_out=s[:, col:col + 1]),
                      rd=[Hs.R(i)], wr=[junk.R(), s.R()])
                kb.op("act", lambda h: h.activation(out=s[:, 2 + col:3 + col], in_=s[:, col:col + 1], func=AF.Sqrt,
                                                    scale=1.0 / n, bias=cx.cst["epsc"][:, 0:1]),
                      rd=[s.R()], wr=[s.R()])
                kb.op("dve", lambda h: h.reciprocal(out=s[:, 4 + col:5 + col], in_=s[:, 2 + col:3 + col]),
                      rd=[s.R()], wr=[s.R()])
                kb.op("dve", lambda h: h.scalar_tensor_tensor(out=cq[:, c0:c0 + n], in0=Hs[:, i, c0:c0 + n],
                                                              scalar=s[:, 4 + col:5 + col], in1=g[:, l, :],
                                                              op0=ALU.mult, op1=ALU.mult),
                      rd=[Hs.R(i), s.R(), g.R()], wr=[cq.R()])
            cx.transpose_to(cq, [cq[:, c * 128:(c + 1) * 128] for c in range(4)],
                            lambda g0, n: cqnT[:, g0:g0 + n, i * 128:(i + 1) * 128], cqnT.R(), BF16)
            cx.transpose_to(cq, [cq[:, 512 + c * 128:512 + (c + 1) * 128] for c in range(2)],
                            lambda g0, n: ckvnT[:, g0:g0 + n, i * 128:(i + 1) * 128], ckvnT.R(), BF16)
            ki = kin.next(); kib = kinb.next()
            kb.op("dve", lambda h: h.tensor_reduce(out=s[:, 6:7], in_=Hs[:, i, 768:832], axis=AX.X, op=ALU.add),
                  rd=[Hs.R(i)], wr=[s.R()])
            kb.op("dve", lambda h: h.tensor_scalar(out=s[:, 7:8], in0=s[:, 6:7], scalar1=-1.0 / 64, scalar2=None,
                                                   op0=ALU.mult), rd=[s.R()], wr=[s.R()])
            kb.op("dve", lambda h: h.tensor_scalar(out=ki[:], in0=Hs[:, i, 768:832], scalar1=s[:, 7:8], scalar2=None,
                                                   op0=ALU.add), rd=[Hs.R(i), s.R()], wr=[ki.R()])
            kb.op("act", lambda h: h.activation(out=junk[:, 0:64], in_=ki[:], func=AF.Square, accum_out=s[:, 8:9]),
                  rd=[ki.R()], wr=[junk.R(), s.R()])
            kb.op("act", lambda h: h.activation(out=s[:, 9:10], in_=s[:, 8:9], func=AF.Sqrt, scale=1.0 / 64,
                                                bias=cx.cst["epsc"][:, 0:1]), rd=[s.R()], wr=[s.R()])
            kb.op("dve", lambda h: h.reciprocal(out=s[:, 10:11], in_=s[:, 9:10]), rd=[s.R()], wr=[s.R()])
            kb.op("dve", lambda h: h.scalar_tensor_tensor(out=ki[:], in0=ki[:], scalar=s[:, 10:11], in1=gki[:, l, :],
                                                          op0=ALU.mult, op1=ALU.mult),
                  rd=[ki.R(), s.R(), gki.R()], wr=[ki.R()])
            kb.op("dve", lambda h: h.tensor_tensor(out=kib[:], in0=ki[:], in1=bki[:, l, :], op=ALU.add),
                  rd=[ki.R(), bki.R()], wr=[kib.R()])
            cx.transpose_to(kib, [kib[:, 0:64]],
                            lambda g0, n: kiT[0:64, i * 128:(i + 1) * 128].rearrange("p (n t) -> p n t", n=1),
                            kiT.R(), BF16, rows=64)
            kb.op("dve", lambda h: h.tensor_scalar(out=wS[:, i, :], in0=Hs[:, i, 832:848], scalar1=1.0 / 32, scalar2=None,
                                                   op0=ALU.mult), rd=[Hs.R(i)], wr=[wS.R(i)])
            kb.op("dve", lambda h: h.tensor_scalar(out=wN[:, i, :], in0=wS[:, i, :], scalar1=0.0, scalar2=None,
                                                   op0=ALU.min), rd=[wS.R(i)], wr=[wN.R(i)])
            kb.op("dve", lambda h: h.tensor_scalar(out=wG[:, i, :], in0=wS[:, i, :], scalar1=0.0, scalar2=2.0,
                                                   op0=ALU.is_ge, op1=ALU.mult), rd=[wS.R(i)], wr=[wG.R()])
            kb.op("dve", lambda h: h.tensor_scalar(out=wG[:, i, :], in0=wG[:, i, :], scalar1=-1.0, scalar2=None,
                                                   op0=ALU.add), rd=[wG.R()], wr=[wG.R()])
        kb.op("sp", lambda h: h.dma_start(out=dk["kiT"].t[:], in_=kiT[:]), rd=[kiT.R()], wr=[dk["kiT"].R()], dma=True)
        kb.op("sp", lambda h: h.dma_start(out=dq["wsg"].t[:], in_=wG[:]), rd=[wG.R()], wr=[dq["wsg"].R()], dma=True)

        qiT = kb.sb(st, "qiT", [128, 9, 1024], BF16)
        qs = Ring([kb.sb(st, "qs", [128, 18, 64], BF16) for _ in range(2)])
        qtmp = kb.sb(st, "qtmp", [128, 16, 64], F32)
        q17 = kb.sb(st, "q17", [128, 64], F32)
        sl_i, sv_i = cx.load_slab(din["w_uqi"].t[0], 4, 1024)
        for i in range(NT):
            q = qs.next()
            if i < 2:
                kb.op("pool", lambda h: h.memset(q[:, 17, :], 0.0), wr=[q.R()])
            b0 = cx.bank(); b1 = cx.bank()
            for half, b in ((0, b0), (1, b1)):
                for k in range(4):
                    kb.op("pe", lambda h: h.matmul(b.t[:, :], lhsT=cqnT[:, k, i * 128:(i + 1) * 128],
                                                   rhs=sv_i[:, k, half * 512:(half + 1) * 512],
                                                   start=(k == 0), stop=(k == 3)),
                          rd=[sl_i.R(), cqnT.R()], wr=[b.R()])
                bv = b.t[:, :].rearrange("p (a d) -> p a d", d=64)
                kb.op("dve", lambda h: h.tensor_tensor(out=q[:, half * 8:(half + 1) * 8, :], in0=bv,
                                                       in1=wS[:, i, half * 8:(half + 1) * 8].unsqueeze(2).to_broadcast([128, 8, 64]),
                                                       op=ALU.mult), rd=[b.R(), wS.R(i)], wr=[q.R()])
                kb.op("dve", lambda h: h.tensor_tensor(out=qtmp[:, half * 8:(half + 1) * 8, :], in0=bv,
                                                       in1=wN[:, i, half * 8:(half + 1) * 8].unsqueeze(2).to_broadcast([128, 8, 64]),
                                                       op=ALU.mult), rd=[b.R(), wN.R(i)], wr=[qtmp.R()])
            kb.op("dve", lambda h: h.tensor_reduce(out=q17[:], in_=qtmp[:].rearrange("p a d -> p d a"), axis=AX.X,
                                                   op=ALU.add), rd=[qtmp.R()], wr=[q17.R()])
            kb.op("dve", lambda h: h.tensor_copy(out=q[:, 16, :], in_=q17[:]), rd=[q17.R()], wr=[q.R()])
            qf = q[:].rearrange("p a d -> p (a d)")
            cx.transpose_to(q, [qf[:, c * 128:(c + 1) * 128] for c in range(9)],
                            lambda g0, n: qiT[:, g0:g0 + n, i * 128:(i + 1) * 128], qiT.R(), BF16)
        kb.op("sp", lambda h: h.dma_start(out=dq["qiT"].t[:], in_=qiT[:]), rd=[qiT.R()], wr=[dq["qiT"].R()], dma=True)

        qaT = fm.next(); kaT = fm.next()
        sl_q, sv_q = cx.load_slab(din["w_uq"].t[0], 4, 768)
        for hh in range(6):
            for tg in range(2):
                b = cx.bank()
                for k in range(4):
                    kb.op("pe", lambda h: h.matmul(b.t[:, :], lhsT=sv_q[:, k, hh * 128:(hh + 1) * 128],
                                                   rhs=cqnT[:, k, tg * 512:(tg + 1) * 512], start=(k == 0), stop=(k == 3)),
                          rd=[sl_q.R(), cqnT.R()], wr=[b.R()])
                cx.copy(cx.evac_eng(), qaT[:, hh, tg * 512:(tg + 1) * 512], b.t[:, :], [b.R()], [qaT.R()])
        kb.op("sp", lambda h: h.dma_start(out=dq["qaT"].t[:], in_=qaT[:]), rd=[qaT.R()], wr=[dq["qaT"].R()], dma=True)
        sl_k, sv_k = cx.load_slab(din["w_ukv"].t[0], 2, 1536)
        for hh in range(6):
            for tg in range(2):
                b = cx.bank()
                for k in range(2):
                    kb.op("pe", lambda h: h.matmul(b.t[:, :], lhsT=sv_k[:, k, hh * 128:(hh + 1) * 128],
                                                   rhs=ckvnT[:, k, tg * 512:(tg + 1) * 512], start=(k == 0), stop=(k == 1)),
                          rd=[sl_k.R(), ckvnT.R()], wr=[b.R()])
                cx.copy(cx.evac_eng(), kaT[:, hh, tg * 512:(tg + 1) * 512], b.t[:, :], [b.R()], [kaT.R()])
        kb.op("sp", lambda h: h.dma_start(out=dk["kaT"].t[:], in_=kaT[:]), rd=[kaT.R()], wr=[dk["kaT"].R()], dma=True)
        for i in range(NT):
            for part, (c0, n) in enumerate(((768, 512), (1280, 256))):
                b = cx.bank()
                for k in range(2):
                    kb.op("pe", lambda h: h.matmul(b.t[:, 0:n], lhsT=ckvnT[:, k, i * 128:(i + 1) * 128],
                                                   rhs=sv_k[:, k, c0:c0 + n], start=(k == 0), stop=(k == 1)),
                          rd=[sl_k.R(), ckvnT.R()], wr=[b.R()])
                h0 = part * 4
                nh = n // 128
                cx.copy(cx.evac_eng(), va[:, i, h0:h0 + nh, 0:128],
                        b.t[:, 0:n].rearrange("p (a d) -> p a d", d=128), [b.R()], [va.R()])
        kb.op("sp", lambda h: h.dma_start(out=dk["va"].t[:], in_=va[:].rearrange("p i h d -> p i (h d)")),
              rd=[va.R()], wr=[dk["va"].R()], dma=True)
    kb.barrier()


def kq_unit(q):
    j, r = q // 8, q % 8
    return r, j, unit_of(r, j)


def phase_B(cx, l, dq, dka, d_attn, mkT, mv):
    kb = cx.kb
    C = cx.cst
    with ExitStack() as st:
        def view_bf16(tile):
            v = Tile(tile.t[:].bitcast(BF16))
            v.res = tile.res
            return v
        kiT2 = view_bf16(cx.stage.tiles[0])
        maskts = view_bf16(cx.stage.tiles[1])
        score = kb.sb(st, "score", [128, 8192], F32)
        maskT = kb.sb(st, "maskT", [128, 64, 256], BF16)
        qi = kb.sb(st, "qi", [128, 9, 256], BF16)
        qa = kb.sb(st, "qa", [128, 6, 256], BF16)
        qb = kb.sb(st, "qb", [128, 6, 256], BF16)
        qc = kb.sb(st, "qc", [128, 4, 256], BF16)
        kmT = kb.sb(st, "kmTa", [128, 8, 4, 6], BF16)
        kring = Ring([kb.sb(st, "ku", [128, 6, 256], BF16) for _ in range(2)])
        vring = Ring([kb.sb(st, "vu", [128, 2, 774], BF16) for _ in range(2)])
        pring = Ring([kb.sb(st, "pT", [128, 128], BF16) for _ in range(4)])
        p2ring = Ring([kb.sb(st, "pT2", [128, 128], BF16) for _ in range(4)])
        cmring = Ring([kb.sb(st, "cm", [128, 256], BF16) for _ in range(2)])
        accO = kb.sb(st, "accO", [128, 12, 129], F32)
        onrm = Ring([kb.sb(st, "onrm", [128, 128], BF16) for _ in range(3)])
        attn_s = kb.sb(st, "attn_s", [128, 16, 256], BF16)
        small = Ring([kb.sb(st, "smB", [128, 16], F32) for _ in range(6)])
        bs = kb.sb(st, "bs", [128, 8], F32)
        gate = kb.sb(st, "gate", [128, 6, 32], F32)
        wsel = kb.sb(st, "wsel", [128, 2, 6, 32], F32)
        vbias = kb.sb(st, "vbias", [128, 32], F32)
        own = kb.sb(st, "own", [128, 32], F32)
        top8 = kb.sb(st, "top8", [128, 8], F32)
        tmp32 = kb.sb(st, "tmp32", [128, 32], F32)
        mb = Ring([kb.sb(st, "mb", [128, 256], F32) for _ in range(2)])
        Mq = kb.sb(st, "Mq", [128, 32], F32)
        sg = kb.sb(st, "sg", [128, 2, 16], F32)
        rlr = Ring([kb.sb(st, "rl", [128, 512], F32) for _ in range(3)])
        pref = kb.sb(st, "pref", [128, 4], F32)
        w6r = Ring([kb.sb(st, "w6", [128, 6], F32) for _ in range(10)])
        e6r = Ring([kb.sb(st, "e6", [128, 6], F32) for _ in range(4)])

        kb.op("sp", lambda h: h.dma_start(out=kmT[:], in_=dka["kmT"].t[:].rearrange("r p j h -> p r j h")),
              rd=[dka["kmT"].R()], wr=[kmT.R()], dma=True)

        for j in range(4):
            NB = 8 * (j + 1)
            NK = NB * 256
            tsl = slice(j * 256, (j + 1) * 256)
            for nm, t_, d_ in (("qiT", qi, dq["qiT"]), ("qaT", qa, dq["qaT"]), ("qbT", qb, dq["qbT"]), ("qcT", qc, dq["qcT"])):
                kb.op("sp", lambda h: h.dma_start(out=t_[:], in_=d_.t[:, :, tsl]), rd=[d_.R()], wr=[t_.R()], dma=True)
            kb.op("sp", lambda h: h.dma_start(out=sg[:], in_=dq["wsg"].t[:, 2 * j:2 * j + 2, :]), rd=[dq["wsg"].R()], wr=[sg.R()], dma=True)
            if True:
                for half in range(2):
                    kb.op("sp", lambda h: h.dma_start(
                        out=kiT2[half * 64:(half + 1) * 64, j * 2048:(j + 1) * 2048].rearrange("p (r s) -> p r s", s=256),
                        in_=dka["kiT"].t[:, :, tsl].rearrange("r p s -> p r s")),
                        rd=[dka["kiT"].R()], wr=[kiT2.R()], dma=True)
            for qt in range(2):
                i = 2 * j + qt
                for c in range(NK // 512):
                    for hh in range(16):
                        b = cx.bank()
                        po = (hh % 2) * 64
                        kb.op("pe", lambda h: h.matmul(b.t[:, :], lhsT=qi[po:po + 64, hh // 2, qt * 128:(qt + 1) * 128],
                                                       rhs=kiT2[po:po + 64, c * 512:(c + 1) * 512], start=True, stop=True),
                              rd=[qi.R(), kiT2.R()], wr=[b.R()])
                        sc = score[:, c * 512:(c + 1) * 512]
                        rl = rlr.next()
                        sgc = sg[:, qt, hh:hh + 1]
                        kb.op("act", lambda h: h.activation(out=rl[:], in_=b.t[:, :], func=AF.Relu, scale=sgc),
                              rd=[b.R(), sg.R()], wr=[rl.R()])
                        if hh == 0:
                            kb.op("dve", lambda h: h.tensor_scalar(out=sc, in0=rl[:], scalar1=sgc, scalar2=None, op0=ALU.mult),
                                  rd=[rl.R(), sg.R()], wr=[score.R()])
                        else:
                            kb.op("dve", lambda h: h.scalar_tensor_tensor(out=sc, in0=rl[:], scalar=sgc, in1=sc,
                                                                          op0=ALU.mult, op1=ALU.add),
                                  rd=[rl.R(), sg.R(), score.R()], wr=[score.R()])
                kb.op("dve", lambda h: h.tensor_reduce(out=bs[:, 5:6], in_=score[:, 0:NK], axis=AX.X, op=ALU.max),
                      rd=[score.R()], wr=[bs.R()])
                kb.op("dve", lambda h: h.tensor_reduce(out=bs[:, 6:7], in_=score[:, 0:NK], axis=AX.X, op=ALU.min),
                      rd=[score.R()], wr=[bs.R()])
                kb.op("dve", lambda h: h.scalar_tensor_tensor(out=bs[:, 5:6], in0=bs[:, 6:7], scalar=-1.0, in1=bs[:, 5:6],
                                                              op0=ALU.mult, op1=ALU.max), rd=[bs.R()], wr=[bs.R()])
                kb.op("dve", lambda h: h.tensor_scalar(out=bs[:, 5:6], in0=bs[:, 5:6], scalar1=1.001, scalar2=1e-6,
                                                       op0=ALU.mult, op1=ALU.add), rd=[bs.R()], wr=[bs.R()])
                kb.op("dve", lambda h: h.tensor_scalar(out=bs[:, 0:1], in0=bs[:, 5:6], scalar1=-1.0, scalar2=None, op0=ALU.mult),
                      rd=[bs.R()], wr=[bs.R()])
                kb.op("dve", lambda h: h.tensor_scalar(out=bs[:, 1:2], in0=bs[:, 5:6], scalar1=2.0, scalar2=None, op0=ALU.mult),
                      rd=[bs.R()], wr=[bs.R()])
                for r in range(8):
                    q = 8 * j + r
                    u = unit_of(r, j)
                    m = mb.next()
                    sm_ = small.next()
                    kb.op("dve", lambda h: h.tensor_scalar(out=sm_[:, 0:1], in0=C["pos_col"][:, i:i + 1], scalar1=float(-256 * u),
                                                           scalar2=None, op0=ALU.add), rd=[C["pos_col"].R()], wr=[sm_.R()])
                    kb.op("dve", lambda h: h.tensor_scalar(out=m[:], in0=C["iota256"][:], scalar1=sm_[:, 0:1], scalar2=NEG,
                                                           op0=ALU.is_gt, op1=ALU.mult), rd=[C["iota256"].R(), sm_.R()], wr=[m.R()])
                    kb.op("dve", lambda h: h.tensor_tensor(out=score[:, q * 256:(q + 1) * 256], in0=score[:, q * 256:(q + 1) * 256],
                                                           in1=m[:], op=ALU.add), rd=[m.R(), score.R()], wr=[score.R()])
                for it in range(N_BISECT):
                    ck = 0.5 ** (it + 1)
                    kb.op("dve", lambda h: h.scalar_tensor_tensor(out=bs[:, 2:3], in0=bs[:, 1:2], scalar=ck, in1=bs[:, 0:1],
                                                                  op0=ALU.mult, op1=ALU.add), rd=[bs.R()], wr=[bs.R()])
                    kb.op("dve", lambda h: h.tensor_scalar(out=maskts[:, 0:NK], in0=score[:, 0:NK], scalar1=bs[:, 2:3], scalar2=None,
                                                           op0=ALU.is_gt, op1=ALU.add, accum_out=bs[:, 3:4]),
                          rd=[score.R(), bs.R()], wr=[maskts.R(), bs.R()])
                    kb.op("dve", lambda h: h.tensor_scalar(out=bs[:, 4:5], in0=bs[:, 3:4], scalar1=255.5, scalar2=bs[:, 1:2],
                                                           op0=ALU.is_ge, op1=ALU.mult), rd=[bs.R()], wr=[bs.R()])
                    kb.op("dve", lambda h: h.scalar_tensor_tensor(out=bs[:, 0:1], in0=bs[:, 4:5], scalar=ck, in1=bs[:, 0:1],
                                                                  op0=ALU.mult, op1=ALU.add), rd=[bs.R()], wr=[bs.R()])
                kb.op("dve", lambda h: h.tensor_scalar(out=maskts[:, 0:NK], in0=score[:, 0:NK], scalar1=bs[:, 0:1], scalar2=None,
                                                       op0=ALU.is_gt), rd=[score.R(), bs.R()], wr=[maskts.R()])
                cx.transpose_to(maskts, [maskts[:, kt * 128:(kt + 1) * 128] for kt in range(NB * 2)],
                                lambda g0, n: maskT[:, g0:g0 + n, qt * 128:(qt + 1) * 128], maskT.R(), BF16)
                for q in range(NB):
                    m = mb.next()
                    kb.op("dve", lambda h: h.scalar_tensor_tensor(out=m[:], in0=score[:, q * 256:(q + 1) * 256], scalar=bs[:, 0:1],
                                                                  in1=C["iota256p1"][:], op0=ALU.is_gt, op1=ALU.mult),
                          rd=[score.R(), bs.R(), C["iota256p1"].R()], wr=[m.R()])
                    kb.op("dve", lambda h: h.tensor_reduce(out=Mq[:, q:q + 1], in_=m[:], axis=AX.X, op=ALU.max), rd=[m.R()], wr=[Mq.R()])
                kb.op("dve", lambda h: h.tensor_scalar(out=tmp32[:, 0:NB], in0=Mq[:, 0:NB], scalar1=0.5, scalar2=None, op0=ALU.is_gt),
                      rd=[Mq.R()], wr=[tmp32.R()])
                kb.op("dve", lambda h: h.tensor_tensor(out=tmp32[:, 0:NB], in0=tmp32[:, 0:NB], in1=C["ucol"][:, 0:NB], op=ALU.mult),
                      rd=[tmp32.R(), C["ucol"].R()], wr=[tmp32.R()])
                kb.op("dve", lambda h: h.tensor_tensor(out=tmp32[:, 0:NB], in0=tmp32[:, 0:NB], in1=Mq[:, 0:NB], op=ALU.add),
                      rd=[tmp32.R(), Mq.R()], wr=[tmp32.R()])
                kb.op("dve", lambda h: h.tensor_reduce(out=pref[:, qt:qt + 1], in_=tmp32[:, 0:NB], axis=AX.X, op=ALU.max),
                      rd=[tmp32.R()], wr=[pref.R()])
                kb.op("dve", lambda h: h.tensor_scalar(out=pref[:, qt:qt + 1], in0=pref[:, qt:qt + 1], scalar1=-1.0, scalar2=None, op0=ALU.add),
                      rd=[pref.R()], wr=[pref.R()])

            kb.op("dve", lambda h: h.tensor_scalar(out=vbias[:], in0=C["ucol"][:], scalar1=C["ustart"][:, j:j + 1], scalar2=NEG,
                                                   op0=ALU.is_ge, op1=ALU.mult), rd=[C["ucol"].R(), C["ustart"].R()], wr=[vbias.R()])
            kb.op("dve", lambda h: h.tensor_scalar(out=own[:], in0=C["ucol"][:], scalar1=C["ustart"][:, j:j + 1], scalar2=None,
                                                   op0=ALU.is_equal), rd=[C["ucol"].R(), C["ustart"].R()], wr=[own.R()])
            for qt in range(2):
                b = cx.bank()
                for hh in range(6):
                    kb.op("pe", lambda h: h.matmul(b.t[:, hh * 32:(hh + 1) * 32], lhsT=qb[:, hh, qt * 128:(qt + 1) * 128],
                                                   rhs=kmT[:, :, :, hh].rearrange("p r j -> p j r"), start=True, stop=True),
                          rd=[qb.R(), kmT.R()], wr=[b.R()])
                kb.op("dve", lambda h: h.tensor_tensor(out=gate[:], in0=b.t[:, 0:192].rearrange("p (a n) -> p a n", n=32),
                                                       in1=vbias[:].unsqueeze(1).to_broadcast([128, 6, 32]), op=ALU.add),
                      rd=[b.R(), vbias.R()], wr=[gate.R()])
                for hh in range(6):
                    kb.op("dve", lambda h: h.max(out=top8[:], in_=gate[:, hh, :]), rd=[gate.R()], wr=[top8.R()])
                    kb.op("dve", lambda h: h.tensor_scalar(out=tmp32[:], in0=gate[:, hh, :], scalar1=top8[:, 2:3], scalar2=None,
                                                           op0=ALU.is_ge), rd=[gate.R(), top8.R()], wr=[tmp32.R()])
                    kb.op("dve", lambda h: h.scalar_tensor_tensor(out=tmp32[:], in0=gate[:, hh, :], scalar=-1e29, in1=tmp32[:],
                                                                  op0=ALU.is_gt, op1=ALU.mult), rd=[gate.R(), tmp32.R()], wr=[tmp32.R()])
                    kb.op("dve", lambda h: h.tensor_tensor(out=wsel[:, qt, hh, :], in0=tmp32[:], in1=own[:], op=ALU.add),
                          rd=[tmp32.R(), own.R()], wr=[wsel.R()])

            for qt in range(2):
                kb.op("dve", lambda h: h.tensor_copy(out=pref[:, 2 + qt:3 + qt], in_=C["pos_col"][:, 2 * j + qt:2 * j + qt + 1]),
                      rd=[C["pos_col"].R()], wr=[pref.R()])
            for kind in range(2):
                kname, vname = ("kaT", "va") if kind == 0 else ("kbT", "vb")
                qT = qa if kind == 0 else qb
                kb.op("pool", lambda h: h.memset(accO[:], 0.0), wr=[accO.R()])
                for q in range(NB):
                    r, jj, u = kq_unit(q)
                    band = (jj == j)
                    ku = kring.next(); vu = vring.next()
                    kb.op("sp", lambda h: h.dma_start(out=ku[:], in_=dka[kname].t[r, :, :, jj * 256:(jj + 1) * 256]),
                          rd=[dka[kname].R()], wr=[ku.R()], dma=True)
                    kb.op("sp", lambda h: h.dma_start(out=vu[:], in_=dka[vname].t[r, :, 2 * jj:2 * jj + 2, :]),
                          rd=[dka[vname].R()], wr=[vu.R()], dma=True)
                    vu4 = vu[:].rearrange("p k (a d) -> p k a d", d=129)
                    wl = {}
                    cml = {}
                    for kt in range(2):
                        for qt in range(2):
                            sm_ = small.next(); e6 = e6r.next(); w6 = w6r.next()
                            ckt = float(256 * u + 128 * kt + 127)
                            pc = kind * 2 + qt
                            kb.op("pool", lambda h: h.tensor_scalar(out=sm_[:, 0:1], in0=pref[:, pc:pc + 1], scalar1=-1.0, scalar2=ckt,
                                                                    op0=ALU.mult, op1=ALU.add), rd=[pref.R()], wr=[sm_.R()])
                            kb.op("pool", lambda h: h.tensor_scalar(out=e6[:], in0=C["slopes"][:, kind * 6:kind * 6 + 6], scalar1=sm_[:, 0:1],
                                                                    scalar2=80.0, op0=ALU.mult, op1=ALU.min),
                                  rd=[C["slopes"].R(), sm_.R()], wr=[e6.R()])
                            kb.op("act", lambda h: h.activation(out=w6[:], in_=e6[:], func=AF.Exp), rd=[e6.R()], wr=[w6.R()])
                            if kind == 1:
                                qcol = jj * 8 + r
                                kb.op("pool", lambda h: h.tensor_tensor(out=w6[:], in0=w6[:], in1=wsel[:, qt, :, qcol], op=ALU.mult),
                                      rd=[w6.R(), wsel.R()], wr=[w6.R()])
                            wl[(kt, qt)] = w6
                        if band and kind == 1:
                            cm = cmring.next(); sm2 = small.next()
                            kb.op("pool", lambda h: h.tensor_scalar(out=sm2[:, 0:1], in0=C["iota_p"][:, 0:1],
                                                                    scalar1=float(256 * u + 128 * kt), scalar2=None, op0=ALU.add),
                                  rd=[C["iota_p"].R()], wr=[sm2.R()])
                            kb.op("pool", lambda h: h.tensor_scalar(out=cm[:], in0=C["pos_row"][:, tsl], scalar1=sm2[:, 0:1], scalar2=None,
                                                                    op0=ALU.is_ge), rd=[C["pos_row"].R(), sm2.R()], wr=[cm.R()])
                            cml[kt] = cm
                    for hh in range(6):
                        ob = [cx.bank(), cx.bank()]
                        for kt in range(2):
                            b = cx.bank()
                            kb.op("pe", lambda h: h.matmul(b.t[:, 0:256], lhsT=ku[:, hh, kt * 128:(kt + 1) * 128], rhs=qT[:, hh, :],
                                                           start=True, stop=True), rd=[ku.R(), qT.R()], wr=[b.R()])
                            for qt in range(2):
                                p = pring.next()
                                kb.op("act", lambda h: h.activation(out=p[:], in_=b.t[:, qt * 128:(qt + 1) * 128], func=AF.Exp,
                                                                    scale=SCALE, bias=C["abias"][:, kind * 6 + hh:kind * 6 + hh + 1]),
                                      rd=[b.R(), C["abias"].R()], wr=[p.R()])
                                p2 = p
                                if kind == 0:
                                    p2 = p2ring.next()
                                    kb.op("pool", lambda h: h.tensor_tensor(out=p2[:], in0=p[:], in1=maskT[:, 2 * q + kt, qt * 128:(qt + 1) * 128],
                                                                            op=ALU.mult), rd=[p.R(), maskT.R()], wr=[p2.R()])
                                elif band:
                                    p2 = p2ring.next()
                                    kb.op("pool", lambda h: h.tensor_tensor(out=p2[:], in0=p[:], in1=cml[kt][:, qt * 128:(qt + 1) * 128],
                                                                            op=ALU.mult), rd=[p.R(), cml[kt].R()], wr=[p2.R()])
                                kb.op("pe", lambda h: h.matmul(ob[kt].t[:, qt * 256:qt * 256 + 129], lhsT=p2[:], rhs=vu4[:, kt, hh, :],
                                                               start=True, stop=True), rd=[p2.R(), vu.R()], wr=[ob[kt].R()])
                        for kt in range(2):
                            for qt in range(2):
                                a_ = accO[:, hh * 2 + qt, :]
                                kb.op("dve", lambda h: h.scalar_tensor_tensor(out=a_, in0=ob[kt].t[:, qt * 256:qt * 256 + 129],
                                                                              scalar=wl[(kt, qt)][:, hh:hh + 1], in1=a_, op0=ALU.mult, op1=ALU.add),
                                      rd=[ob[kt].R(), accO.R(), wl[(kt, qt)].R()], wr=[accO.R()])
                for hh in range(6):
                    for qt in range(2):
                        sm_ = small.next(); o_ = onrm.next()
                        a_ = accO[:, hh * 2 + qt, :]
                        kb.op("dve", lambda h: h.reciprocal(out=sm_[:, 0:1], in_=a_[:, 128:129]), rd=[accO.R()], wr=[sm_.R()])
                        kb.op("dve", lambda h: h.tensor_scalar(out=o_[:], in0=a_[:, 0:128], scalar1=sm_[:, 0:1], scalar2=None, op0=ALU.mult),
                              rd=[accO.R(), sm_.R()], wr=[o_.R()])
                        cx.transpose_to(o_, [o_[:]], lambda g0, n: attn_s[:, kind * 6 + hh, qt * 128:(qt + 1) * 128].rearrange("p (n t) -> p n t", n=1),
                                        attn_s.R(), BF16)
            mv4 = mv
            for hc in range(4):
                ob = [cx.bank(), cx.bank()]
                for mt in range(2):
                    b = cx.bank()
                    kb.op("pe", lambda h: h.matmul(b.t[:, 0:256], lhsT=mkT[:, hc, mt * 128:(mt + 1) * 128], rhs=qc[:, hc, :],
                                                   start=True, stop=True), rd=[mkT.R(), qc.R()], wr=[b.R()])
                    for qt in range(2):
                        p = pring.next()
                        kb.op("act", lambda h: h.activation(out=p[:], in_=b.t[:, qt * 128:(qt + 1) * 128], func=AF.Exp, scale=SCALE),
                              rd=[b.R()], wr=[p.R()])
                        kb.op("pe", lambda h: h.matmul(ob[qt].t[:, 0:129], lhsT=p[:], rhs=mv4[:, mt, hc, :],
                                                       start=(mt == 0), stop=(mt == 1)), rd=[p.R(), mv.R()], wr=[ob[qt].R()])
                for qt in range(2):
                    sm_ = small.next(); o_ = onrm.next()
                    kb.op("dve", lambda h: h.reciprocal(out=sm_[:, 0:1], in_=ob[qt].t[:, 128:129]), rd=[ob[qt].R()], wr=[sm_.R()])
                    kb.op("dve", lambda h: h.tensor_scalar(out=o_[:], in0=ob[qt].t[:, 0:128], scalar1=sm_[:, 0:1], scalar2=None, op0=ALU.mult),
                          rd=[ob[qt].R(), sm_.R()], wr=[o_.R()])
                    cx.transpose_to(o_, [o_[:]], lambda g0, n: attn_s[:, 12 + hc, qt * 128:(qt + 1) * 128].rearrange("p (n t) -> p n t", n=1),
                                    attn_s.R(), BF16)
            kb.op("sp", lambda h: h.dma_start(out=d_attn.t[:, :, tsl], in_=attn_s[:]), rd=[attn_s.R()], wr=[d_attn.R()], dma=True)
    kb.barrier()


def layer_norm_tile(cx, x_ap, x_res, gb, gb_res, sm_, junk):
    kb = cx.kb
    kb.op("dve", lambda h: h.tensor_reduce(out=sm_[:, 0:1], in_=x_ap, axis=AX.X, op=ALU.add), rd=[x_res], wr=[sm_.R()])
    kb.op("dve", lambda h: h.tensor_scalar(out=sm_[:, 1:2], in0=sm_[:, 0:1], scalar1=-1.0 / 2048, scalar2=None, op0=ALU.mult),
          rd=[sm_.R()], wr=[sm_.R()])
    kb.op("dve", lambda h: h.tensor_scalar(out=x_ap, in0=x_ap, scalar1=sm_[:, 1:2], scalar2=None, op0=ALU.add),
          rd=[x_res, sm_.R()], wr=[x_res])
    kb.op("act", lambda h: h.activation(out=junk[:], in_=x_ap, func=AF.Square, accum_out=sm_[:, 2:3]),
          rd=[x_res], wr=[junk.R(), sm_.R()])
    kb.op("act", lambda h: h.activation(out=sm_[:, 3:4], in_=sm_[:, 2:3], func=AF.Sqrt, scale=1.0 / 2048,
                                        bias=cx.cst["epsc"][:, 0:1]), rd=[sm_.R()], wr=[sm_.R()])
    kb.op("dve", lambda h: h.reciprocal(out=sm_[:, 4:5], in_=sm_[:, 3:4]), rd=[sm_.R()], wr=[sm_.R()])
    kb.op("dve", lambda h: h.scalar_tensor_tensor(out=x_ap, in0=x_ap, scalar=sm_[:, 4:5], in1=gb[:, 0:2048],
                                                  op0=ALU.mult, op1=ALU.mult), rd=[x_res, sm_.R(), gb_res], wr=[x_res])
    kb.op("pool", lambda h: h.tensor_tensor(out=x_ap, in0=x_ap, in1=gb[:, 2048:4096], op=ALU.add),
          rd=[x_res, gb_res], wr=[x_res])


def load_gb(cx, dg, db, l):
    kb = cx.kb
    gb = cx.stage.next()
    kb.op("sp", lambda h: h.dma_start(out=gb[:, 0:2048], in_=dg.t[:, l, :]), wr=[gb.R()], dma=True)
    kb.op("sp", lambda h: h.dma_start(out=gb[:, 2048:4096], in_=db.t[:, l, :]), wr=[gb.R()], dma=True)
    return gb


def phase_C(cx, l, xs_d, d_attn, din, d_x1pre, d_out):
    kb = cx.kb
    C = cx.cst
    with ExitStack() as st:
        smr = Ring([kb.sb(st, "smC", [128, 16], F32) for _ in range(4)])
        with ExitStack() as s1:
            xT = kb.sb(s1, "xTc", [128, 16, 1024], BF16)
            attnT = kb.sb(s1, "attnT", [128, 16, 1024], BF16)
            mergedT = kb.sb(s1, "mergedT", [128, 16, 1024], BF16)
            gtr = Ring([kb.sb(s1, "gts", [128, 2, 1024], BF16) for _ in range(2)])
            macc = kb.sb(s1, "macc", [128, 4, 512], F32)
            tmpr = Ring([kb.sb(s1, "tmpm", [128, 512], F32) for _ in range(1)])
            xblk = Ring([kb.sb(s1, "xblk", [128, 256], F32) for _ in range(2)])
            kb.op("sp", lambda h: h.dma_start(out=attnT[:], in_=d_attn.t[:]), rd=[d_attn.R()], wr=[attnT.R()], dma=True)
            for i in range(NT):
                xi = cx.stage.next()
                kb.op("sp", lambda h: h.dma_start(out=xi[:, 0:2048], in_=xs_d.t[i * 128:(i + 1) * 128, :]),
                      rd=[xs_d.R()], wr=[xi.R()], dma=True)
                cx.transpose_to(xi, [xi[:, c * 128:(c + 1) * 128] for c in range(16)],
                                lambda g0, n: xT[:, g0:g0 + n, i * 128:(i + 1) * 128], xT.R(), F32)
            w_gate = din["w_gate"].t[0]
            ups = ((din["w_up_a"].t[0], 6, 0), (din["w_up_b"].t[0], 6, 6), (din["w_up_c"].t[0], 4, 12))
            for fp in range(8):
                for gi, (wu, nk, off) in enumerate(ups):
                    gt = gtr.next()
                    sl, sv = cx.load_slab(w_gate[:, gi * 2048 + fp * 256: gi * 2048 + (fp + 1) * 256], 16, 256)
                    for fc2 in range(2):
                        for tg in range(2):
                            b = cx.bank()
                            for k in range(16):
                                kb.op("pe", lambda h: h.matmul(b.t[:, :], lhsT=sv[:, k, fc2 * 128:(fc2 + 1) * 128],
                                                               rhs=xT[:, k, tg * 512:(tg + 1) * 512], start=(k == 0), stop=(k == 15)),
                                      rd=[sl.R(), xT.R()], wr=[b.R()])
                            col = gi * 16 + fp * 2 + fc2
                            kb.op("act", lambda h: h.activation(out=gt[:, fc2, tg * 512:(tg + 1) * 512], in_=b.t[:, :],
                                                                func=AF.Sigmoid, bias=C["b_gate"][:, l, col:col + 1]),
                                  rd=[b.R(), C["b_gate"].R()], wr=[gt.R()])
                    sl, sv = cx.load_slab(wu[:, fp * 256:(fp + 1) * 256], nk, 256)
                    for fc2 in range(2):
                        for tg in range(2):
                            b = cx.bank()
                            for k in range(nk):
                                kb.op("pe", lambda h: h.matmul(b.t[:, :], lhsT=sv[:, k, fc2 * 128:(fc2 + 1) * 128],
                                                               rhs=attnT[:, off + k, tg * 512:(tg + 1) * 512], start=(k == 0), stop=(k == nk - 1)),
                                      rd=[sl.R(), attnT.R()], wr=[b.R()])
                            g_ap = gt[:, fc2, tg * 512:(tg + 1) * 512]
                            ma = macc[:, fc2 * 2 + tg, :]
                            mres = macc.R(fc2 * 2 + tg)
                            if gi == 0:
                                kb.op("dve", lambda h: h.tensor_tensor(out=ma, in0=b.t[:, :], in1=g_ap, op=ALU.mult),
                                      rd=[b.R(), gt.R()], wr=[mres])
                            else:
                                tm = tmpr.next()
                                kb.op("dve", lambda h: h.tensor_tensor(out=tm[:], in0=b.t[:, :], in1=g_ap, op=ALU.mult),
                                      rd=[b.R(), gt.R()], wr=[tm.R()])
                                if gi == 1:
                                    kb.op("pool", lambda h: h.tensor_tensor(out=ma, in0=ma, in1=tm[:], op=ALU.add),
                                          rd=[tm.R(), mres], wr=[mres])
                                else:
                                    kb.op("pool", lambda h: h.tensor_tensor(out=mergedT[:, fp * 2 + fc2, tg * 512:(tg + 1) * 512],
                                                                            in0=ma, in1=tm[:], op=ALU.add),
                                          rd=[tm.R(), mres], wr=[mergedT.R()])
            w_o = din["w_o"].t[0]
            for cs in range(8):
                sl, sv = cx.load_slab(w_o[:, cs * 256:(cs + 1) * 256], 16, 256)
                for i in range(NT):
                    b = cx.bank()
                    for k in range(16):
                        kb.op("pe", lambda h: h.matmul(b.t[:, 0:256], lhsT=mergedT[:, k, i * 128:(i + 1) * 128], rhs=sv[:, k, :],
                                                       start=(k == 0), stop=(k == 15)), rd=[sl.R(), mergedT.R()], wr=[b.R()])
                    xb = xblk.next()
                    kb.op("sp", lambda h: h.dma_start(out=xb[:], in_=xs_d.t[i * 128:(i + 1) * 128, cs * 256:(cs + 1) * 256]),
                          rd=[xs_d.R()], wr=[xb.R()], dma=True)
                    kb.op("dve", lambda h: h.scalar_tensor_tensor(out=xb[:], in0=xb[:], scalar=ALPHA, in1=b.t[:, 0:256],
                                                                  op0=ALU.mult, op1=ALU.add), rd=[xb.R(), b.R()], wr=[xb.R()])
                    kb.op("sp", lambda h: h.dma_start(out=d_x1pre.t[i * 128:(i + 1) * 128, cs * 256:(cs + 1) * 256], in_=xb[:]),
                          rd=[xb.R()], wr=[d_x1pre.R()], dma=True)
            kb.barrier()
        x1T = kb.sb(st, "x1T", [128, 16, 1024], BF16)
        acc = kb.sb(st, "acc", [128, 8, 2048], F32)
        comb = kb.sb(st, "comb", [128, 8, 32], F32)
        hidr = Ring([kb.sb(st, "hid", [128, 4, 1024], BF16) for _ in range(2)])
        silall = kb.sb(st, "silall", [128, 4, 1024], BF16)
        lg = kb.sb(st, "lg", [128, 36], F32)
        rt = kb.sb(st, "rt", [128, 64], F32)
        top8 = kb.sb(st, "top8c", [128, 8], F32)
        junk = Tile(hidr.tiles[0].t[:, 0:2, :])
        junk.res = hidr.tiles[0].res
        gb = load_gb(cx, din["ln1_g"], din["ln1_b"], l)
        for i in range(NT):
            xa = acc[:, i, :]
            kb.op("sp", lambda h: h.dma_start(out=xa, in_=d_x1pre.t[i * 128:(i + 1) * 128, :]), rd=[d_x1pre.R()], wr=[acc.R(i)], dma=True)
            layer_norm_tile(cx, xa, acc.R(i), gb, gb.R(), smr.next(), junk)
            for g0 in range(0, 16, 4):
                b = cx.bank()
                for q in range(4):
                    kb.op("pe", lambda h: h.transpose(out=b.t[:, q * 128:(q + 1) * 128], in_=acc[:, i, (g0 + q) * 128:(g0 + q + 1) * 128],
                                                      identity=C["ident"][:]), rd=[acc.R(i), C["ident"].R()], wr=[b.R()])
                cx.copy(cx.evac_eng(), x1T[:, g0:g0 + 4, i * 128:(i + 1) * 128], b.t[:, :].rearrange("p (n t) -> p n t", t=128),
                        [b.R()], [x1T.R()])
        slr, svr = cx.load_slab(din["w_r"].t[0], 16, 36)
        for i in range(NT):
            b = cx.bank()
            for k in range(16):
                kb.op("pe", lambda h: h.matmul(b.t[:, 0:36], lhsT=x1T[:, k, i * 128:(i + 1) * 128], rhs=svr[:, k, :],
                                               start=(k == 0), stop=(k == 15)), rd=[slr.R(), x1T.R()], wr=[b.R()])
            R_ = [lg.R(), rt.R(), top8.R()]
            def dv(fn, rd=R_, wr=R_):
                kb.op("dve", fn, rd=rd, wr=wr)
            dv(lambda h: h.tensor_tensor(out=lg[:], in0=b.t[:, 0:36], in1=C["b_r"][:, l, :], op=ALU.add), rd=[b.R(), C["b_r"].R()] + R_)
            dv(lambda h: h.tensor_reduce(out=rt[:, 0:1], in_=lg[:, 0:4], axis=AX.X, op=ALU.max))
            dv(lambda h: h.tensor_scalar(out=rt[:, 4:8], in0=lg[:, 0:4], scalar1=rt[:, 0:1], scalar2=None, op0=ALU.is_equal))
            dv(lambda h: h.tensor_scalar(out=rt[:, 1:2], in0=rt[:, 0:1], scalar1=-1.0, scalar2=None, op0=ALU.mult))
            kb.op("act", lambda h: h.activation(out=rt[:, 8:12], in_=lg[:, 0:4], func=AF.Exp, bias=rt[:, 1:2], accum_out=rt[:, 2:3]),
                  rd=R_, wr=R_)
            dv(lambda h: h.reciprocal(out=rt[:, 3:4], in_=rt[:, 2:3]))
            dv(lambda h: h.tensor_scalar(out=rt[:, 16:24], in0=lg[:, 4:12], scalar1=rt[:, 4:5], scalar2=None, op0=ALU.mult))
            for g in range(1, 4):
                dv(lambda h: h.scalar_tensor_tensor(out=rt[:, 16:24], in0=lg[:, 4 + 8 * g:12 + 8 * g], scalar=rt[:, 4 + g:5 + g],
                                                    in1=rt[:, 16:24], op0=ALU.mult, op1=ALU.add))
            dv(lambda h: h.max(out=top8[:], in_=rt[:, 16:24]))
            dv(lambda h: h.tensor_tensor(out=rt[:, 12:13], in0=top8[:, 1:2], in1=top8[:, 0:1], op=ALU.subtract))
            kb.op("act", lambda h: h.activation(out=rt[:, 13:14], in_=rt[:, 12:13], func=AF.Exp), rd=R_, wr=R_)
            dv(lambda h: h.tensor_scalar(out=rt[:, 14:15], in0=rt[:, 13:14], scalar1=1.0, scalar2=None, op0=ALU.add))
            dv(lambda h: h.reciprocal(out=rt[:, 14:15], in_=rt[:, 14:15]))
            dv(lambda h: h.tensor_tensor(out=rt[:, 15:16], in0=rt[:, 13:14], in1=rt[:, 14:15], op=ALU.mult))
            dv(lambda h: h.tensor_scalar(out=rt[:, 24:32], in0=rt[:, 16:24], scalar1=top8[:, 0:1], scalar2=rt[:, 14:15],
                                         op0=ALU.is_equal, op1=ALU.mult))
            dv(lambda h: h.tensor_scalar(out=rt[:, 32:40], in0=rt[:, 16:24], scalar1=top8[:, 1:2], scalar2=rt[:, 15:16],
                                         op0=ALU.is_equal, op1=ALU.mult))
            dv(lambda h: h.tensor_tensor(out=rt[:, 24:32], in0=rt[:, 24:32], in1=rt[:, 32:40], op=ALU.add))
            dv(lambda h: h.tensor_scalar(out=rt[:, 24:32], in0=rt[:, 24:32], scalar1=rt[:, 3:4], scalar2=None, op0=ALU.mult))
            for g in range(4):
                kb.op("dve", lambda h: h.tensor_scalar(out=comb[:, i, 8 * g:8 * g + 8], in0=rt[:, 24:32], scalar1=rt[:, 4 + g:5 + g],
                                                       scalar2=None, op0=ALU.mult), rd=R_, wr=[comb.R()])
            kb.op("pool", lambda h: h.tensor_scalar(out=acc[:, i, :], in0=acc[:, i, :], scalar1=ALPHA, scalar2=None, op0=ALU.mult),
                  rd=[acc.R(i)], wr=[acc.R(i)])
        for e in range(32):
            hid = hidr.next()
            w1e = din["w1"].t[0, e]; w3e = din["w3"].t[0, e]; w2e = din["w2"].t[0, e]
            for which, wsrc, wres in ((0, w1e, din["w1"].R()), (1, w3e, din["w3"].R())):
                sA, vA = cx.load_slab(wsrc[0:1024, :], 8, 512)
                sB, vB = cx.load_slab(wsrc[1024:2048, :], 8, 512)
                for fc in range(4):
                    for tg in range(2):
                        bb = cx.bank()
                        for k in range(16):
                            ss_, vv_ = (sA, vA) if k < 8 else (sB, vB)
                            kb.op("pe", lambda h: h.matmul(bb.t[:, :], lhsT=vv_[:, k % 8, fc * 128:(fc + 1) * 128],
                                                           rhs=x1T[:, k, tg * 512:(tg + 1) * 512], start=(k == 0), stop=(k == 15)),
                                  rd=[ss_.R(), x1T.R()], wr=[bb.R()])
                        if which == 0:
                            kb.op("act", lambda h: h.activation(out=silall[:, fc, tg * 512:(tg + 1) * 512], in_=bb.t[:, :], func=AF.Silu),
                                  rd=[bb.R()], wr=[silall.R()])
                        else:
                            kb.op("dve", lambda h: h.tensor_tensor(out=hid[:, fc, tg * 512:(tg + 1) * 512], in0=bb.t[:, :],
                                                                   in1=silall[:, fc, tg * 512:(tg + 1) * 512], op=ALU.mult),
                                  rd=[bb.R(), silall.R()], wr=[hid.R()])
            for half in range(2):
                s2_, v2 = cx.load_slab(w2e[:, half * 1024:(half + 1) * 1024], 4, 1024)
                for i in range(NT):
                    for cc in range(2):
                        b = cx.bank()
                        for k in range(4):
                            kb.op("pe", lambda h: h.matmul(b.t[:, :], lhsT=hid[:, k, i * 128:(i + 1) * 128], rhs=v2[:, k, cc * 512:(cc + 1) * 512],
                                                           start=(k == 0), stop=(k == 3)), rd=[s2_.R(), hid.R()], wr=[b.R()])
                        c0 = half * 1024 + cc * 512
                        kb.op("dve", lambda h: h.scalar_tensor_tensor(out=acc[:, i, c0:c0 + 512], in0=b.t[:, :], scalar=comb[:, i, e:e + 1],
                                                                      in1=acc[:, i, c0:c0 + 512], op0=ALU.mult, op1=ALU.add),
                              rd=[b.R(), comb.R(), acc.R(i)], wr=[acc.R(i)])
        gb = load_gb(cx, din["ln2_g"], din["ln2_b"], l)
        for i in range(NT):
            layer_norm_tile(cx, acc[:, i, :], acc.R(i), gb, gb.R(), smr.next(), junk)
            kb.op("sp", lambda h: h.dma_start(out=d_out.t[i * 128:(i + 1) * 128, :], in_=acc[:, i, :]), rd=[acc.R(i)], wr=[d_out.R()], dma=True)
    kb.barrier()


CONSTS_A = {"ident": ([128, 128], F32), "epsc": ([128, 1], F32),
            "g_cq": ([128, DEPTH, 512], F32), "g_ckv": ([128, DEPTH, 256], F32),
            "g_kidx": ([128, DEPTH, 64], F32), "b_kidx": ([128, DEPTH, 64], F32)}
CONSTS_BC = {"ident": ([128, 128], F32), "epsc": ([128, 1], F32),
             "pos_col": ([128, 8], F32), "cpos": ([128, 8], F32), "pos_row": ([128, 1024], F32),
             "iota256": ([128, 256], F32), "iota_p": ([128, 1], F32), "slopes": ([128, 12], F32),
             "ucol": ([128, 32], F32), "ustart": ([128, 4], F32), "abias": ([128, 12], F32), "iota256p1": ([128, 256], F32),
             "b_gate": ([128, DEPTH, 48], F32), "b_r": ([128, DEPTH, 36], F32)}

QSHAPES = {"qiT": [128, 9, 1024], "qaT": [128, 6, 1024], "qbT": [128, 6, 1024], "qcT": [128, 4, 1024]}
KSHAPES = {"kiT": [64, 1024], "kaT": [128, 6, 1024], "va": [128, 8, 774], "kbT": [128, 6, 1024],
           "vb": [128, 8, 774], "kmT": [128, 4, 6]}
WSH = {"w_in": [1, 2048, 3664], "w_uq": [1, 512, 768], "w_uqi": [1, 512, 1024],
       "w_ukv": [1, 256, 1536], "w_up_a": [1, 768, 2048], "w_up_b": [1, 768, 2048],
       "w_up_c": [1, 512, 2048], "w_gate": [1, 2048, 6144], "w_o": [1, 2048, 2048],
       "w1": [1, 32, 2048, 512], "w3": [1, 32, 2048, 512], "w2": [1, 32, 512, 2048],
       "w_mem_kv": [2048, 1024], "mem": [256, 2048], "w_r": [1, 2048, 36],
       "ln1_g": [128, DEPTH, 2048], "ln1_b": [128, DEPTH, 2048], "ln2_g": [128, DEPTH, 2048], "ln2_b": [128, DEPTH, 2048]}
A_W = ("w_in", "w_uq", "w_uqi", "w_ukv")
BC_W = ("w_up_a", "w_up_b", "w_up_c", "w_gate", "w_o", "w1", "w3", "w2", "w_mem_kv", "mem", "w_r",
        "ln1_g", "ln1_b", "ln2_g", "ln2_b")


def build_A(l):
    kb = KB()
    with ExitStack() as stack:
        cx = Ctx(kb, stack, CONSTS_A)
        din = {n: kb.dram(n, WSH[n], F32, "ExternalInput") for n in A_W}
        xs = kb.dram("xs", [TL, D], F32, "ExternalInput")
        dq = {n: kb.dram(n, s, BF16, "ExternalOutput") for n, s in QSHAPES.items()}
        dq["wsg"] = kb.dram("wsg", [128, 8, 16], F32, "ExternalOutput")
        dk = {n: kb.dram(n, s, BF16, "ExternalOutput") for n, s in KSHAPES.items()}
        phase_A(cx, l, xs, din, dq, dk)
        kb.final_wait()
    return kb


def build_BC(l, with_next_A, stop_after=None, debug=False):
    kb = KB()
    with ExitStack() as stack:
        consts = dict(CONSTS_BC)
        if with_next_A:
            consts.update(CONSTS_A)
        cx = Ctx(kb, stack, consts)
        names = BC_W + (A_W if with_next_A else ())
        din = {n: kb.dram(n, WSH[n], F32, "ExternalInput") for n in names}
        xs = kb.dram("xs", [TL, D], F32, "ExternalInput")
        dq = {n: kb.dram(n + "_in", s, BF16, "ExternalInput") for n, s in QSHAPES.items()}
        dq["wsg"] = kb.dram("wsg_in", [128, 8, 16], F32, "ExternalInput")
        dka = {n: kb.dram(n + "_all", [8] + s, BF16, "ExternalInput") for n, s in KSHAPES.items()}
        d_attn = kb.dram("attn_scr", [128, 16, 1024], BF16, "ExternalOutput" if (stop_after == "B" or debug) else "Internal")
        if stop_after != "B":
            d_x1pre = kb.dram("x1pre_scr", [TL, D], F32, "ExternalOutput" if debug else "Internal")
        d_out = kb.dram("xout", [TL, D], F32, "ExternalOutput")
        mkT, mv = phase_M(cx, stack, din)
        phase_B(cx, l, dq, dka, d_attn, mkT, mv)
        if stop_after != "B":
            phase_C(cx, l, xs, d_attn, din, d_x1pre, d_out)
            if with_next_A:
                dq2 = {n: kb.dram(n, s, BF16, "ExternalOutput") for n, s in QSHAPES.items()}
                dq2["wsg"] = kb.dram("wsg", [128, 8, 16], F32, "ExternalOutput")
                dk2 = {n: kb.dram(n, s, BF16, "ExternalOutput") for n, s in KSHAPES.items()}
                phase_A(cx, l + 1, d_out, din, dq2, dk2)
        kb.final_wait()
    return kb


def core_rows(c):
    return np.concatenate([np.arange(unit_of(c, j) * 256, unit_of(c, j) * 256 + 256) for j in range(4)])


def bcast(v):
    v = np.asarray(v, dtype=np.float32)
    return np.ascontiguousarray(np.broadcast_to(v[None], (128,) + v.shape))


def host_consts(inp, c):
    rows = core_rows(c).astype(np.float32)
    slopes = 2.0 ** (-8.0 * np.arange(1, 13, dtype=np.float32) / 12)
    sl12 = np.concatenate([slopes[0::2], slopes[1::2]]).astype(np.float32)
    ucol = np.array([256.0 * unit_of(q % 8, q // 8) for q in range(32)], np.float32)
    ustart = np.array([256.0 * unit_of(c, j) for j in range(4)], np.float32)
    b_r = np.concatenate([inp["b_grp"], inp["b_rt"]], axis=-1)
    cst = {
        "ident": np.eye(128, dtype=np.float32), "epsc": np.full((128, 1), EPS, np.float32),
        "g_cq": bcast(inp["g_cq"]), "g_ckv": bcast(inp["g_ckv"]), "g_kidx": bcast(inp["g_kidx"]), "b_kidx": bcast(inp["b_kidx"]),
        "pos_col": np.ascontiguousarray(rows.reshape(8, 128).T), "cpos": bcast(rows.reshape(8, 128)[:, 0] + 64.0),
        "pos_row": bcast(rows), "iota256": bcast(np.arange(256, dtype=np.float32)),
        "iota_p": np.arange(128, dtype=np.float32).reshape(128, 1), "slopes": bcast(sl12),
        "ucol": bcast(ucol), "ustart": bcast(ustart),
        "abias": np.ascontiguousarray((np.arange(128, dtype=np.float32)[:, None] - 127.0) * sl12[None, :]),
        "iota256p1": bcast(np.arange(1, 257, dtype=np.float32)),
        "b_gate": np.ascontiguousarray(np.asarray(inp["b_gate"], np.float32).reshape(DEPTH, 48, 128).transpose(2, 0, 1)),
        "b_r": bcast(b_r),
        "ln1_g": bcast(inp["ln1_g"]), "ln1_b": bcast(inp["ln1_b"]), "ln2_g": bcast(inp["ln2_g"]), "ln2_b": bcast(inp["ln2_b"]),
    }
    return cst


def layer_weights(inp, l):
    w = {n: np.asarray(inp[n][l:l + 1], np.float32) for n in ("w_in", "w_uq", "w_uqi", "w_ukv", "w_up_a", "w_up_b", "w_up_c",
                                                              "w_gate", "w_o", "w1", "w3", "w2")}
    w["w_r"] = np.concatenate([inp["w_grp"][l:l + 1], inp["w_rt"][l:l + 1]], axis=-1).astype(np.float32)
    w["w_mem_kv"] = np.asarray(inp["w_mem_kv"], np.float32)
    w["mem"] = np.asarray(inp["mem"][0], np.float32)
    return w


_CACHE = {}


def get_prog(key, fn):
    if key not in _CACHE:
        _CACHE[key] = fn()
    return _CACHE[key]


def run_A(inp, l, xs_list):
    kb = get_prog(("A", l), lambda: build_A(l))
    w = layer_weights(inp, l)
    maps = []
    for c in range(NCORE):
        cst = host_consts(inp, c)
        m = {n: cst[n] for n in CONSTS_A}
        m.update({n: w[n] for n in A_W})
        m["xs"] = xs_list[c]
        maps.append(m)
    res = run_bass_kernel_spmd(kb.nc, maps, core_ids=list(range(NCORE)))
    return res.results


def run_BC(inp, l, xs_list, qk, with_next_A, stop_after=None):
    kb = get_prog(("BC", l, with_next_A, stop_after), lambda: build_BC(l, with_next_A, stop_after))
    w = layer_weights(inp, l)
    if with_next_A:
        w.update({n: np.asarray(inp[n][l + 1:l + 2], np.float32) for n in A_W})
    kall = {n + "_all": np.ascontiguousarray(np.stack([np.asarray(qk[c][n]) for c in range(NCORE)])) for n in KSHAPES}
    maps = []
    for c in range(NCORE):
        cst = host_consts(inp, c)
        m = {n: cst[n] for n in CONSTS_BC}
        if with_next_A:
            m.update({n: cst[n] for n in CONSTS_A})
        for n in BC_W + (A_W if with_next_A else ()):
            m[n] = cst[n] if n.startswith("ln") else w[n]
        m["xs"] = xs_list[c]
        for n in QSHAPES:
            m[n + "_in"] = np.asarray(qk[c][n])
        m["wsg_in"] = np.asarray(qk[c]["wsg"])
        m.update(kall)
        maps.append(m)
    res = run_bass_kernel_spmd(kb.nc, maps, core_ids=list(range(NCORE)))
    return res.results


def kernel(**inp):
    x = np.asarray(inp["x"], np.float32)[0]
    xs_list = [np.ascontiguousarray(x[core_rows(c)]) for c in range(NCORE)]
    r = run_A(inp, 0, xs_list)
    r = run_BC(inp, 0, xs_list, r, True)
    xs_list = [np.asarray(r[c]["xout"]) for c in range(NCORE)]
    r = run_BC(inp, 1, xs_list, r, False)
    out = np.zeros((1, T, D), np.float32)
    for c in range(NCORE):
        out[0, core_rows(c)] = np.asarray(r[c]["xout"])
    return out
```
